# Optimizing a Trainium2 kernel written in Bass

```python
import jax, jax.numpy as jnp
from jax import lax
import numpy as np

D_MODEL = 1024
BATCH = 4
SEQ = 4096
DEPTH = 2

HEAD_DIM = 64
N_MIXERS = 4
GROUP_WIDTH = D_MODEL // N_MIXERS
GROUP_HEADS = GROUP_WIDTH // HEAD_DIM
D_MIX = N_MIXERS * GROUP_WIDTH
Q_BLOCK = 128
ROPE_THETA = 500000.0
ROPE_FRACTION = 4
EPS = 1e-6
NEG = -1e30

IDX_HEADS = 4
IDX_DIM = 32
DSA_TOPK = 256

CHUNK = 128

POOL_WINDOWS = (2, 4, 8, 16)
POOL_CH = GROUP_WIDTH // len(POOL_WINDOWS)

KV_DIM = HEAD_DIM
CMP_BLOCK = 32
CMP_STRIDE = 16
SEL_BLOCK = 64
SEL_TOPN = 16
WINDOW = 512

N_EXPERTS = 16
N_EXPERT_GROUPS = 4
EXPERTS_PER_GROUP = N_EXPERTS // N_EXPERT_GROUPS
TOP_K = 2
D_EXPERT = 256

PROJ_SPLITS = (GROUP_WIDTH, GROUP_WIDTH, GROUP_WIDTH, IDX_HEADS * IDX_DIM, IDX_DIM, IDX_HEADS,
               GROUP_WIDTH, GROUP_WIDTH,
               GROUP_WIDTH,
               GROUP_WIDTH, KV_DIM, KV_DIM, KV_DIM, KV_DIM, KV_DIM, KV_DIM, GROUP_HEADS * 3)
IN_WIDTH = sum(PROJ_SPLITS)

kernel_name = 'hybrid_dsa_gmlp_pool_nsa_moe'


def rmsnorm(x, g):
    xf = x.astype(jnp.float32)
    y = xf * lax.rsqrt(jnp.mean(xf * xf, axis=-1, keepdims=True) + EPS)
    return y.astype(x.dtype) * g


def partial_rope(x, pos):
    rd = x.shape[-1] // ROPE_FRACTION
    half = rd // 2
    inv_freq = ROPE_THETA ** (-jnp.arange(half, dtype=jnp.float32) / half)
    ang = pos.astype(jnp.float32)[:, :, None, None] * inv_freq
    cos, sin = jnp.cos(ang), jnp.sin(ang)
    xr = x[..., :rd].astype(jnp.float32)
    x1, x2 = xr[..., :half], xr[..., half:]
    rot = jnp.concatenate([x1 * cos - x2 * sin, x2 * cos + x1 * sin], axis=-1).astype(x.dtype)
    return jnp.concatenate([rot, x[..., rd:]], axis=-1)


def unblock(o):
    o = jnp.moveaxis(o, 0, 1)
    return o.reshape((o.shape[0], o.shape[1] * o.shape[2]) + o.shape[3:])


def gather_rows(table, idx):
    return jax.vmap(lambda t, i: t[i])(table, idx)


def split_projection(z):
    points = np.cumsum(np.array(PROJ_SPLITS))[:-1].tolist()
    return jnp.split(z, points, axis=-1)


def dsa_mixer(q, k, v, iq, ik, iw, pos):
    bsz, seq = q.shape[0], q.shape[1]
    topk = min(DSA_TOPK, seq // 4)
    q = partial_rope(q, pos)
    k = partial_rope(k, pos)
    iq = partial_rope(iq, pos).astype(jnp.float32)
    ik = partial_rope(ik[:, :, None, :], pos)[:, :, 0].astype(jnp.float32)
    iw = iw.astype(jnp.float32)
    key_pos = jnp.arange(seq)
    scale = HEAD_DIM ** -0.5

    def block(i):
        s0 = i * Q_BLOCK
        t_pos = s0 + jnp.arange(Q_BLOCK)
        qb = lax.dynamic_slice_in_dim(q, s0, Q_BLOCK, 1)
        iqb = lax.dynamic_slice_in_dim(iq, s0, Q_BLOCK, 1)
        iwb = lax.dynamic_slice_in_dim(iw, s0, Q_BLOCK, 1)
        idx_logits = jax.nn.relu(jnp.einsum('bthd,bsd->bths', iqb, ik))
        score = jnp.einsum('bth,bths->bts', iwb, idx_logits)
        causal = key_pos[None, :] <= t_pos[:, None]
        score = jnp.where(causal[None], score, -jnp.inf)
        _, sel = lax.top_k(score, topk)
        valid = sel <= t_pos[None, :, None]
        k_sel = gather_rows(k, sel)
        v_sel = gather_rows(v, sel)
        logits = jnp.einsum('bthd,btkhd->bhtk', qb, k_sel).astype(jnp.float32) * scale
        logits = jnp.where(valid[:, None], logits, -jnp.inf)
        p = jax.nn.softmax(logits, axis=-1).astype(v.dtype)
        return jnp.einsum('bhtk,btkhd->bthd', p, v_sel)

    out = unblock(lax.map(block, jnp.arange(seq // Q_BLOCK)))
    return out.reshape(bsz, seq, GROUP_WIDTH)


def gmlp_mixer(u, v, norm_g, w_s, b_s):
    bsz, seq = u.shape[0], u.shape[1]
    u = jax.nn.gelu(u)
    v = jax.nn.gelu(v)
    vf = v.astype(jnp.float32)
    mu = jnp.mean(vf, axis=-1, keepdims=True)
    var = jnp.mean(jnp.square(vf - mu), axis=-1, keepdims=True)
    v = ((vf - mu) * lax.rsqrt(var + EPS)).astype(u.dtype) * norm_g
    v = v.reshape(bsz, seq // CHUNK, CHUNK, GROUP_HEADS, HEAD_DIM)
    w_causal = jnp.where(jnp.tril(jnp.ones((CHUNK, CHUNK), dtype=bool)), w_s, 0)
    mixed = jnp.einsum('hts,bnshd->bnthd', w_causal, v) + jnp.swapaxes(b_s, 0, 1)[:, :, None]
    return u * mixed.reshape(bsz, seq, GROUP_WIDTH)


def pool_mixer(z, w_pool, scale):
    bsz, seq = z.shape[0], z.shape[1]
    zf = z.astype(jnp.float32).reshape(bsz, seq, len(POOL_WINDOWS), POOL_CH)
    csum = jnp.concatenate([jnp.zeros_like(zf[:, :1]), jnp.cumsum(zf, axis=1)], axis=1)
    t = jnp.arange(seq)
    pooled = []
    for g, w in enumerate(POOL_WINDOWS):
        lo = jnp.maximum(t + 1 - w, 0)
        count = (t + 1 - lo).astype(jnp.float32)[None, :, None]
        window_sum = csum[:, 1:, g] - csum[:, lo, g]
        pooled.append(window_sum / count - zf[:, :, g])
    pooled = jnp.stack(pooled, axis=2).astype(z.dtype)
    y = jnp.einsum('blgc,gce->blge', pooled, w_pool)
    return y.reshape(bsz, seq, GROUP_WIDTH) * scale


def nsa_mixer(q, kc, vc, ks, vs, kw, vw, gate_logits, w_cmp1, w_cmp2, cmp_pe, pos):
    bsz, seq = q.shape[0], q.shape[1]
    n_cmp = (seq - CMP_BLOCK) // CMP_STRIDE + 1
    n_sel = seq // SEL_BLOCK
    top_n = min(SEL_TOPN, n_sel)
    scale = HEAD_DIM ** -0.5
    t_all = jnp.arange(seq)
    q = partial_rope(q, pos)

    cmp_start = jnp.arange(n_cmp) * CMP_STRIDE
    cmp_idx = cmp_start[:, None] + jnp.arange(CMP_BLOCK)[None, :]

    def compress(kv, w1, w2, pe):
        blocks = kv[:, cmp_idx] + pe
        return jax.nn.gelu(jnp.einsum('bnjd,jde->bne', blocks, w1)) @ w2

    k_cmp = compress(kc, w_cmp1[0], w_cmp2[0], cmp_pe[0])
    v_cmp = compress(vc, w_cmp1[1], w_cmp2[1], cmp_pe[1])
    k_cmp = partial_rope(k_cmp[:, :, None], pos[:, cmp_start + CMP_BLOCK // 2])[:, :, 0]
    cmp_visible = (cmp_start + CMP_BLOCK - 1)[None, :] <= t_all[:, None]
    logits = jnp.einsum('blhd,bnd->bhln', q, k_cmp).astype(jnp.float32) * scale
    p_cmp = jax.nn.softmax(jnp.where(cmp_visible, logits, NEG), axis=-1)
    p_cmp = jnp.where(cmp_visible.any(-1)[:, None], p_cmp, 0.0)
    o_cmp = jnp.einsum('bhln,bnd->blhd', p_cmp.astype(vc.dtype), v_cmp)

    sel_start = jnp.arange(n_sel) * SEL_BLOCK
    overlap = jnp.clip(jnp.minimum(sel_start[:, None] + SEL_BLOCK, cmp_start[None, :] + CMP_BLOCK)
                       - jnp.maximum(sel_start[:, None], cmp_start[None, :]), 0)
    overlap = overlap.astype(jnp.float32) / CMP_BLOCK
    importance = jnp.einsum('bhln,jn->blj', p_cmp, overlap)
    blk = jnp.arange(n_sel)
    admissible = sel_start[None, :] <= t_all[:, None]
    forced = (blk[None, :] == (t_all // SEL_BLOCK)[:, None]) | (blk[None, :] == 0)
    importance = jnp.where(forced, jnp.inf, importance)
    importance = jnp.where(admissible, importance, -jnp.inf)
    _, sel_idx = lax.top_k(importance, top_n)
    sel_valid = sel_idx * SEL_BLOCK <= t_all[None, :, None]

    ks = partial_rope(ks[:, :, None], pos)[:, :, 0].reshape(bsz, n_sel, SEL_BLOCK, KV_DIM)
    vs = vs.reshape(bsz, n_sel, SEL_BLOCK, KV_DIM)
    kw = partial_rope(kw[:, :, None], pos)[:, :, 0]
    kw_pad = jnp.pad(kw, ((0, 0), (WINDOW, 0), (0, 0)))
    vw_pad = jnp.pad(vw, ((0, 0), (WINDOW, 0), (0, 0)))
    in_block = jnp.arange(SEL_BLOCK)
    win_off = jnp.arange(WINDOW + Q_BLOCK) - WINDOW

    def block(i):
        s0 = i * Q_BLOCK
        t_pos = s0 + jnp.arange(Q_BLOCK)
        qb = lax.dynamic_slice_in_dim(q, s0, Q_BLOCK, 1)
        idx = lax.dynamic_slice_in_dim(sel_idx, s0, Q_BLOCK, 1)
        ok = lax.dynamic_slice_in_dim(sel_valid, s0, Q_BLOCK, 1)
        k_g = gather_rows(ks, idx).reshape(bsz, Q_BLOCK, top_n * SEL_BLOCK, KV_DIM)
        v_g = gather_rows(vs, idx).reshape(bsz, Q_BLOCK, top_n * SEL_BLOCK, KV_DIM)
        key_pos = idx[..., None] * SEL_BLOCK + in_block
        mask = (ok[..., None] & (key_pos <= t_pos[None, :, None, None])).reshape(bsz, Q_BLOCK, -1)
        logits_s = jnp.einsum('bthd,btkd->bhtk', qb, k_g).astype(jnp.float32) * scale
        logits_s = jnp.where(mask[:, None], logits_s, -jnp.inf)
        o_sel = jnp.einsum('bhtk,btkd->bthd', jax.nn.softmax(logits_s, axis=-1).astype(vs.dtype), v_g)
        k_win = lax.dynamic_slice_in_dim(kw_pad, s0, WINDOW + Q_BLOCK, 1)
        v_win = lax.dynamic_slice_in_dim(vw_pad, s0, WINDOW + Q_BLOCK, 1)
        kpos = s0 + win_off
        wmask = ((kpos[None, :] <= t_pos[:, None]) & (kpos[None, :] > t_pos[:, None] - WINDOW)
                 & (kpos[None, :] >= 0))
        logits_w = jnp.einsum('bthd,bsd->bhts', qb, k_win).astype(jnp.float32) * scale
        logits_w = jnp.where(wmask, logits_w, -jnp.inf)
        o_win = jnp.einsum('bhts,bsd->bthd', jax.nn.softmax(logits_w, axis=-1).astype(vw.dtype), v_win)
        return o_sel, o_win

    o_sel, o_win = lax.map(block, jnp.arange(seq // Q_BLOCK))
    o_sel = unblock(o_sel)
    o_win = unblock(o_win)
    g = jax.nn.sigmoid(gate_logits.astype(jnp.float32)).astype(q.dtype)
    out = g[..., 0:1] * o_cmp + g[..., 1:2] * o_sel + g[..., 2:3] * o_win
    return out.reshape(bsz, seq, GROUP_WIDTH)


def moe(h, router_w, router_b, w_gate, w_up, w_down):
    bsz, seq, d = h.shape
    tok = h.reshape(-1, d)
    affinity = jax.nn.sigmoid((tok @ router_w).astype(jnp.float32))
    biased = affinity + router_b.astype(jnp.float32)
    group_score = lax.top_k(biased.reshape(-1, N_EXPERT_GROUPS, EXPERTS_PER_GROUP), TOP_K)[0].sum(-1)
    best_group = jnp.argmax(group_score, axis=-1)
    in_group = (jnp.arange(N_EXPERTS) // EXPERTS_PER_GROUP)[None, :] == best_group[:, None]
    _, top_idx = lax.top_k(jnp.where(in_group, biased, -jnp.inf), TOP_K)
    top_aff = jnp.take_along_axis(affinity, top_idx, axis=-1)
    weights = top_aff / jnp.sum(top_aff, axis=-1, keepdims=True)
    gate = jnp.sum(jax.nn.one_hot(top_idx, N_EXPERTS, dtype=jnp.float32) * weights[..., None], axis=1)
    gate = gate.astype(h.dtype)
    out = jnp.zeros_like(tok)
    for e in range(N_EXPERTS):
        hidden = jax.nn.silu(tok @ w_gate[e]) * (tok @ w_up[e])
        out = out + gate[:, e:e + 1] * (hidden @ w_down[e])
    return out.reshape(bsz, seq, d)


def hybrid_layer(x, c_act, pos, ada_w, ada_b, g_mix, g_ffn, w_in, sgu_g, w_sgu, b_sgu, w_pool, pool_scale,
                 w_cmp1, w_cmp2, cmp_pe, w_out, router_w, router_b, w_gate, w_up, w_down):
    bsz, seq = x.shape[0], x.shape[1]
    mod = c_act @ ada_w + ada_b
    shift_m, scale_m, gate_m, shift_f, scale_f, gate_f = [m[:, None, :] for m in jnp.split(mod, 6, axis=-1)]

    h = rmsnorm(x, g_mix) * (1 + scale_m) + shift_m
    (a_q, a_k, a_v, a_iq, a_ik, a_iw, b_u, b_v, c_z,
     d_q, d_kc, d_vc, d_ks, d_vs, d_kw, d_vw, d_g) = split_projection(h @ w_in)
    o_a = dsa_mixer(a_q.reshape(bsz, seq, GROUP_HEADS, HEAD_DIM), a_k.reshape(bsz, seq, GROUP_HEADS, HEAD_DIM),
                    a_v.reshape(bsz, seq, GROUP_HEADS, HEAD_DIM), a_iq.reshape(bsz, seq, IDX_HEADS, IDX_DIM),
                    a_ik, a_iw, pos)
    o_b = gmlp_mixer(b_u, b_v, sgu_g, w_sgu, b_sgu)
    o_c = pool_mixer(c_z, w_pool, pool_scale)
    o_d = nsa_mixer(d_q.reshape(bsz, seq, GROUP_HEADS, HEAD_DIM), d_kc, d_vc, d_ks, d_vs, d_kw, d_vw,
                    d_g.reshape(bsz, seq, GROUP_HEADS, 3), w_cmp1, w_cmp2, cmp_pe, pos)
    mixed = jnp.concatenate([o_a, o_b, o_c, o_d], axis=-1) @ w_out
    x = x + gate_m * mixed

    h = rmsnorm(x, g_ffn) * (1 + scale_f) + shift_f
    return x + gate_f * moe(h, router_w, router_b, w_gate, w_up, w_down)


def setup_inputs(seed: int = 0) -> dict:
    key = jax.random.key(seed)
    ks = jax.random.split(key, 24)

    def nrm(k, shape, s):
        return jax.random.normal(k, shape, jnp.float32) * s

    def gain(k, shape):
        return 1.0 + nrm(k, shape, 0.02)

    return {
        'x': nrm(ks[0], (BATCH, SEQ, D_MODEL), 1.0),
        'c': nrm(ks[1], (BATCH, D_MODEL), 1.0),
        'positions': jnp.arange(SEQ, dtype=jnp.int32)[None, :]
                     + jax.random.randint(ks[2], (BATCH, 1), 0, 1024, dtype=jnp.int32),
        'ada_w': nrm(ks[3], (DEPTH, D_MODEL, 6 * D_MODEL), 0.5 * D_MODEL ** -0.5),
        'ada_b': nrm(ks[4], (DEPTH, 6 * D_MODEL), 0.02),
        'norm_mix_g': gain(ks[5], (DEPTH, D_MODEL)),
        'norm_ffn_g': gain(ks[6], (DEPTH, D_MODEL)),
        'w_in': nrm(ks[7], (DEPTH, D_MODEL, IN_WIDTH), D_MODEL ** -0.5),
        'sgu_norm_g': gain(ks[8], (DEPTH, GROUP_WIDTH)),
        'w_sgu': nrm(ks[9], (DEPTH, GROUP_HEADS, CHUNK, CHUNK), CHUNK ** -0.5),
        'b_sgu': gain(ks[10], (DEPTH, GROUP_HEADS, CHUNK)),
        'w_pool': nrm(ks[11], (DEPTH, len(POOL_WINDOWS), POOL_CH, POOL_CH), POOL_CH ** -0.5),
        'pool_scale': 1.0 + nrm(ks[12], (DEPTH, GROUP_WIDTH), 0.1),
        'w_cmp1': nrm(ks[13], (DEPTH, 2, CMP_BLOCK, KV_DIM, KV_DIM), (CMP_BLOCK * KV_DIM) ** -0.5),
        'w_cmp2': nrm(ks[14], (DEPTH, 2, KV_DIM, KV_DIM), KV_DIM ** -0.5),
        'cmp_pe': nrm(ks[15], (DEPTH, 2, CMP_BLOCK, KV_DIM), 0.1),
        'w_out': nrm(ks[16], (DEPTH, D_MIX, D_MODEL), D_MIX ** -0.5),
        'router_w': nrm(ks[17], (D_MODEL, N_EXPERTS), D_MODEL ** -0.5),
        'router_b': nrm(ks[18], (N_EXPERTS,), 0.01),
        'w_gate': nrm(ks[19], (DEPTH, N_EXPERTS, D_MODEL, D_EXPERT), D_MODEL ** -0.5),
        'w_up': nrm(ks[20], (DEPTH, N_EXPERTS, D_MODEL, D_EXPERT), D_MODEL ** -0.5),
        'w_down': nrm(ks[21], (DEPTH, N_EXPERTS, D_EXPERT, D_MODEL), D_EXPERT ** -0.5),
        'final_norm_g': gain(ks[22], (D_MODEL,)),
    }


def reference(x, c, positions, ada_w, ada_b, norm_mix_g, norm_ffn_g, w_in, sgu_norm_g, w_sgu, b_sgu, w_pool,
              pool_scale, w_cmp1, w_cmp2, cmp_pe, w_out, router_w, router_b, w_gate, w_up, w_down, final_norm_g):
    c_act = jax.nn.silu(c)
    for layer in range(DEPTH):
        x = hybrid_layer(x, c_act, positions, ada_w[layer], ada_b[layer], norm_mix_g[layer], norm_ffn_g[layer],
                         w_in[layer], sgu_norm_g[layer], w_sgu[layer], b_sgu[layer], w_pool[layer],
                         pool_scale[layer], w_cmp1[layer], w_cmp2[layer], cmp_pe[layer], w_out[layer],
                         router_w, router_b, w_gate[layer], w_up[layer], w_down[layer])
    return rmsnorm(x, final_norm_g)
```

```python
import numpy as np
from contextlib import ExitStack
import concourse.bass as bass
import concourse.mybir as mybir
from concourse.bass_utils import run_bass_kernel_spmd

F32 = mybir.dt.float32
BF16 = mybir.dt.bfloat16
U8 = mybir.dt.uint8
I32 = mybir.dt.int32
AF = mybir.ActivationFunctionType
ALU = mybir.AluOpType
AX = mybir.AxisListType

S = 4096
D = 1024
NT = 32
INW = 2352
EPS = 1e-6
NEGB = -32768.0
BIG = 1.0e30
SCALE = 0.125
N_LANES = 24

C_AQ, C_AK, C_AV, C_IQ, C_IK, C_IW = 0, 256, 512, 768, 896, 928
C_BU, C_BV, C_CZ = 932, 1188, 1444
C_DQ, C_KC, C_VC, C_KS, C_VS, C_KW, C_VW, C_DG = 1700, 1956, 2020, 2084, 2148, 2212, 2276, 2340


class _Op:
    __slots__ = ("eng", "fn", "reads", "writes", "lane", "idx", "deps", "signal", "seq", "is_dma")

    def __init__(self, eng, fn, reads, writes, lane):
        self.eng = eng
        self.fn = fn
        self.reads = reads
        self.writes = writes
        self.lane = lane
        self.is_dma = lane is not None
        self.deps = []
        self.signal = False
        self.seq = 0


class Prog:
    def __init__(self, nc, n_lanes):
        self.nc = nc
        self.ops = []
        self.n_lanes = n_lanes
        self._rr = 0
        self._rr_pool = 0
        self.barriers = []
        self._cap = None

    def add(self, eng, fn, reads=(), writes=(), lane=None):
        op = _Op(eng, fn, tuple(reads), tuple(writes), lane)
        (self._cap if self._cap is not None else self.ops).append(op)
        return op

    def capture(self):
        self._cap = []

    def end_capture(self):
        c, self._cap = self._cap, None
        return c

    def emit_merged(self, lists):
        keyed = []
        for li, lst in enumerate(lists):
            n = len(lst)
            for j, o in enumerate(lst):
                keyed.append(((j + 0.5) / n, li, j, o))
        keyed.sort(key=lambda t: (t[0], t[1], t[2]))
        for _, _, _, o in keyed:
            self.ops.append(o)

    def dma(self, eng, fn, reads=(), writes=()):
        half = self.n_lanes // 2
        if eng == "pool":
            lane = half + self._rr_pool
            self._rr_pool = (self._rr_pool + 1) % (self.n_lanes - half)
        else:
            lane = self._rr
            self._rr = (self._rr + 1) % half
        return self.add(eng, fn, reads, writes, lane)

    def track(self, op):
        return ("lane", op.lane) if op.is_dma else op.eng

    def barrier(self):
        self.barriers.append(len(self.ops))

    def finalize(self, sems):
        last_w, readers, lane_last = {}, {}, {}
        ops = self.ops
        last_on_track = {}
        pending = {}
        bank_last = {}
        bset = set(self.barriers)
        for n_, op in enumerate(ops):
            op.idx = n_
        for op in ops:
            deps = set()
            if op.idx in bset:
                snap = list(last_on_track.values())
                for en in ("pe", "act", "dve", "pool", "sp"):
                    pending[en] = snap
            if pending.get(op.eng):
                deps.update(pending[op.eng])
                pending[op.eng] = None
            for r in op.reads:
                if r in last_w:
                    deps.add(last_w[r])
            for w in op.writes:
                if w in last_w:
                    deps.add(last_w[w])
                for rd in readers.get(w, ()):
                    deps.add(rd)
            if op.is_dma and op.lane in lane_last:
                deps.add(lane_last[op.lane])
            bks = {k[:5] for k in op.reads + op.writes if k.startswith("bank")}
            for bkey in bks:
                d_ = bank_last.setdefault(bkey, {})
                for en, oi in d_.items():
                    if en != op.eng:
                        deps.add(oi)
                d_[op.eng] = op.idx
            deps.discard(op.idx)
            op.deps = [d for d in deps if not (op.eng == "pe" and not op.is_dma
                                               and ops[d].eng == "pe" and not ops[d].is_dma)]
            for r in op.reads:
                readers.setdefault(r, []).append(op.idx)
            for w in op.writes:
                last_w[w] = op.idx
                readers[w] = []
            if op.is_dma:
                lane_last[op.lane] = op.idx
            last_on_track[self.track(op)] = op.idx
        for op in ops:
            for d in op.deps:
                ops[d].signal = True
        cnt = {}
        for op in ops:
            t = self.track(op)
            if op.is_dma:
                op.signal = True
            if op.signal:
                cnt[t] = cnt.get(t, 0) + 1
                op.seq = cnt[t]
        self.sems = sems
        self.final_counts = cnt

    def emit_engine(self, ename, eobj, final_wait=False):
        ops, sems = self.ops, self.sems
        seen = {}
        for op in ops:
            if op.eng != ename:
                continue
            need = {}
            for d in op.deps:
                dop = ops[d]
                t = self.track(dop)
                v = dop.seq * (16 if dop.is_dma else 1)
                if need.get(t, 0) < v:
                    need[t] = v
            for t, v in need.items():
                if seen.get(t, 0) >= v:
                    continue
                eobj.wait_ge(sems[t], v)
                seen[t] = v
            ins = op.fn(eobj)
            if op.signal:
                ins.then_inc(sems[self.track(op)], 16 if op.is_dma else 1)
        if final_wait:
            for t, c in self.final_counts.items():
                v = c * (16 if isinstance(t, tuple) else 1)
                if seen.get(t, 0) < v:
                    eobj.wait_ge(sems[t], v)


class Arena:
    def __init__(self, t, nbytes):
        self.t = t
        self.nbytes = nbytes
        self.off = 0
        self.peak = 0

    def alloc(self, shape, dt):
        n = 1
        for s in shape:
            n *= s
        nb = n * mybir.dt.size(dt)
        nb_al = (nb + 63) // 64 * 64
        assert self.off + nb_al <= self.nbytes, f"arena overflow {self.off}+{nb_al}>{self.nbytes}"
        ap = self.t[:, self.off:self.off + nb].bitcast(dt)
        self.off += nb_al
        self.peak = max(self.peak, self.off)
        if len(shape) == 2:
            ap = ap.rearrange("p (a b) -> p a b", a=shape[0], b=shape[1])
        elif len(shape) == 3:
            ap = ap.rearrange("p (a b c) -> p a b c", a=shape[0], b=shape[1], c=shape[2])
        return ap

    def mark(self):
        return self.off

    def release(self, m):
        self.off = m


def _constants():
    k = {}
    t = np.arange(128)
    k["k_ident"] = np.eye(128, dtype=np.float32)
    k["k_nident"] = -np.eye(128, dtype=np.float32)
    k["k_cb"] = np.where(t[None, :] <= t[:, None], 0.0, NEGB).astype(np.float32)
    k["k_wb"] = np.where(t[None, :] > t[:, None], 0.0, NEGB).astype(np.float32)
    k["k_trilT"] = (t[None, :] >= t[:, None]).astype(np.float32)
    wins = (2, 4, 8, 16)
    mc = np.zeros((4, 128, 128), np.float32)
    mp = np.zeros((4, 128, 128), np.float32)
    m0 = np.zeros((4, 128, 128), np.float32)
    for g, w in enumerate(wins):
        for tt in range(128):
            for ss in range(max(0, tt - w + 1), tt + 1):
                mc[g, ss, tt] += 1.0 / w
            for back in range(tt + 1, w):
                mp[g, 128 - (back - tt), tt] += 1.0 / w
            cnt = min(tt + 1, w)
            for ss in range(max(0, tt - w + 1), tt + 1):
                m0[g, ss, tt] += 1.0 / cnt
            mc[g, tt, tt] -= 1.0
            m0[g, tt, tt] -= 1.0
    k["k_mcur"] = mc.transpose(1, 0, 2).copy()
    k["k_mprev"] = mp.transpose(1, 0, 2).copy()
    k["k_m0"] = m0.transpose(1, 0, 2).copy()
    k["k_d0"] = (t[None, :] - 16.0 * t[:, None]).astype(np.float32)
    n = np.arange(256)
    j = np.arange(64)
    ov = np.clip(np.minimum(j[:, None] * 64 + 64, n[None, :] * 16 + 32) - np.maximum(j[:, None] * 64, n[None, :] * 16), 0, None)
    ov = ov.astype(np.float32) / 32.0
    ov[:, 255] = 0.0
    k["k_ovT"] = ov.T.copy().reshape(2, 128, 64).transpose(1, 0, 2).copy()
    inv64 = (500000.0 ** (-np.arange(8, dtype=np.float32) / 8.0)).astype(np.float32)
    inv32 = (500000.0 ** (-np.arange(4, dtype=np.float32) / 4.0)).astype(np.float32)
    k["k_invf"] = np.tile(np.concatenate([inv64, inv32])[None, :], (128, 1)).astype(np.float32)
    sel = np.zeros((16, 16, 128), np.float32)
    for e in range(16):
        sel[e, e, :] = 1.0
    k["k_sel16"] = sel
    hm2 = np.zeros((128, 2, 128), np.float32)
    hm2[:64, 0, :] = 1.0
    hm2[64:, 1, :] = 1.0
    k["k_hm2"] = hm2
    hm4 = np.zeros((128, 4, 128), np.float32)
    for h in range(4):
        hm4[32 * h:32 * h + 32, h, :] = 1.0
    k["k_hm4"] = hm4
    k["k_pow2"] = np.tile((0.5 ** np.arange(1, 33, dtype=np.float32))[None, :], (128, 1)).astype(np.float32)
    return k


def build_program(n_tiles=NT, n_layers=2, debug=(), cut=None):
    nc = bass.Bass("TRN2", target_bir_lowering=False)

    def din(name, shape, dt=F32):
        return nc.dram_tensor(name, list(shape), dt, kind="ExternalInput").ap()

    def dscr(name, shape, dt):
        return nc.dram_tensor(name, list(shape), dt, kind="Internal").ap()

    def dout(name, shape, dt=F32):
        return nc.dram_tensor(name, list(shape), dt, kind="ExternalOutput").ap()

    x_d = din("x", [S, D])
    c_d = din("c2", [128, 8])
    pos_d = din("pos2", [128, 32], I32)
    posc_d = din("posc2", [128, 2], I32)
    adaw_d = din("ada_w", [2, D, 6 * D])
    adab_d = din("ada_b", [2, 6 * D])
    gmix_d = din("norm_mix_g", [2, D])
    gffn_d = din("norm_ffn_g", [2, D])
    gfin_d = din("final_norm_g", [1, D])
    win_d = din("w_in", [2, D, INW])
    sgug_d = din("sgu_norm_g", [2, 256])
    wsguT_d = din("w_sguT", [2, 128, 4, 128])
    bsgu_d = din("b_sguT", [2, 128, 4])
    wpool_d = din("w_pool2", [2, 64, 4, 64])
    pscale_d = din("pool_scale", [2, 256])
    wc1_d = din("w_cmp1", [2, 2, 32, 64, 64])
    wc2_d = din("w_cmp2", [2, 2, 64, 64])
    peT_d = din("cmp_peT", [2, 2, 64, 32])
    wout_d = din("w_out", [2, D, D])
    rw_d = din("router_w", [D, 16])
    rb_d = din("router_b", [1, 16])
    wg_d = din("w_gate", [2, 16, D, 256])
    wu_d = din("w_up", [2, 16, D, 256])
    wd_d = din("w_down", [2, 16, 256, D])
    kd = {}
    for name, arr in _constants().items():
        kd[name] = din(name, arr.shape)
    y_d = dout("y", [S, D])
    dbg = {}
    if "z" in debug:
        dbg["z"] = dout("dbg_z", [n_tiles * 128, INW])
    if "obc" in debug:
        dbg["obc"] = dout("dbg_obc", [n_tiles * 128, 512], BF16)
    if "cat" in debug:
        dbg["cat"] = dout("dbg_cat", [n_tiles * 128, D], BF16)
    if "xmid" in debug:
        dbg["xmid"] = dout("dbg_xmid", [n_tiles * 128, D])
    if "mba" in debug:
        dbg["mba"] = dout("dbg_mba", [128, S], BF16)
        dbg["sst"] = dout("dbg_sst", [128, 64])
        dbg["cz"] = dout("dbg_cz", [128, S], mybir.dt.float16)
        dbg["score"] = dout("dbg_score", [128, S])
    if "gate" in debug:
        dbg["gate"] = dout("dbg_gate", [n_tiles * 128, 16], BF16)
    if "xout" in debug:
        dbg["xout"] = dout("dbg_xout", [n_tiles * 128, D])
    if "mod" in debug:
        dbg["mod"] = dout("dbg_mod", [128, 6 * D])

    xs_d = dscr("xs", [2, S, D], F32)
    qscr_d = dscr("qscr", [NT, 128, 1536], BF16)
    cat_d = dscr("catscr", [NT, 128, 1024], BF16)

    es = ExitStack()
    with es:
        ARENA_BYTES = 206 * 1024
        arena_t = es.enter_context(nc.sbuf_tensor("arena", [128, ARENA_BYTES], U8))
        ar = Arena(arena_t, ARENA_BYTES)
        banks = [es.enter_context(nc.psum_tensor(f"bank{i}", [128, 512], F32)) for i in range(8)]

        def bk(i):
            return banks[i][:, :]

        def bkb(i):
            return banks[i][:, :].bitcast(BF16)

        P = Prog(nc, N_LANES)

        def op(eng, meth, r=(), w=(), **kw):
            P.add(eng, lambda e: getattr(e, meth)(**kw), r, w)

        def dma(eng, out, in_, r=(), w=()):
            P.dma(eng, lambda e: e.dma_start(out=out, in_=in_), r, w)

        marks = {}

        def mark(name):
            marks.setdefault(name, len(P.ops))

        def flat(ap):
            if len(ap.shape) == 3:
                return ap.rearrange("p a b -> p (a b)")
            return ap.rearrange("p a b c -> p (a b c)")

        ident_b = ar.alloc([128], BF16)
        ident_f = ar.alloc([128], F32)
        cb_b = ar.alloc([128], BF16)
        wb_b = ar.alloc([128], BF16)
        trilT = ar.alloc([128], F32)
        mcur = ar.alloc([4, 128], BF16)
        mprev = ar.alloc([4, 128], BF16)
        m0 = ar.alloc([4, 128], BF16)
        d0 = ar.alloc([128], F32)
        ovT = ar.alloc([2, 64], BF16)
        invf = ar.alloc([12], F32)
        pow2 = ar.alloc([32], F32)
        hm2 = ar.alloc([2, 128], BF16)
        hm4 = ar.alloc([4, 128], BF16)
        dma("pool", hm2, kd["k_hm2"][:, :, :], w=["hm2"])
        dma("pool", hm4, kd["k_hm4"][:, :, :], w=["hm4"])
        dma("pool", ident_b, kd["k_ident"][:, :], w=["ident_b"])
        nident_b = ar.alloc([128], BF16)
        negh = ar.alloc([1], F32)
        op("pool", "memset", ap=negh, constant=-0.5, w=["negh"])
        dma("pool", nident_b, kd["k_nident"][:, :], w=["nident_b"])
        dma("sp", ident_f, kd["k_ident"][:, :], w=["ident_f"])
        dma("pool", cb_b, kd["k_cb"][:, :], w=["cb_b"])
        dma("pool", wb_b, kd["k_wb"][:, :], w=["wb_b"])
        dma("sp", trilT, kd["k_trilT"][:, :], w=["trilT"])
        dma("pool", mcur, kd["k_mcur"][:, :, :], w=["mcur"])
        dma("pool", mprev, kd["k_mprev"][:, :, :], w=["mprev"])
        dma("pool", m0, kd["k_m0"][:, :, :], w=["m0"])
        dma("sp", d0, kd["k_d0"][:, :], w=["d0"])
        dma("pool", ovT, kd["k_ovT"][:, :, :], w=["ovT"])
        dma("sp", invf, kd["k_invf"][:, :], w=["invf"])
        dma("sp", pow2, kd["k_pow2"][:, :], w=["pow2"])

        sincos = ar.alloc([32, 24], F32)
        sincosc = ar.alloc([2, 16], F32)
        m_tmp = ar.mark()
        pos_i = ar.alloc([34], I32)
        pos_f = ar.alloc([34], F32)
        ang = ar.alloc([34, 12], F32)
        angs = ar.alloc([34, 24], F32)
        kq_i = ar.alloc([34, 24], I32)
        kq_f = ar.alloc([34, 24], F32)
        red = ar.alloc([34, 24], F32)
        wrp = ar.alloc([34, 24], F32)
        sc_all = ar.alloc([34, 24], F32)
        dma("sp", pos_i[:, 0:32], pos_d[:, :], w=["pos_i"])
        dma("sp", pos_i[:, 32:34], posc_d[:, :], w=["pos_ic"])
        op("dve", "tensor_copy", out=pos_f, in_=pos_i, r=["pos_i", "pos_ic"], w=["pos_f"])
        op("dve", "tensor_tensor", out=ang, in0=pos_f.unsqueeze(2).to_broadcast([128, 34, 12]),
           in1=invf.unsqueeze(1).to_broadcast([128, 34, 12]), op=ALU.mult, r=["pos_f", "invf"], w=["ang"])
        TWO_PI = float(2 * np.pi)
        op("dve", "tensor_scalar", out=angs[:, :, 0:12], in0=ang, scalar1=float(np.pi / 2), scalar2=None, op0=ALU.add,
           r=["ang"], w=["angs_c"])
        op("dve", "tensor_copy", out=angs[:, :, 12:24], in_=ang, r=["ang"], w=["angs_s"])
        op("dve", "tensor_scalar", out=kq_f, in0=angs, scalar1=float(1.0 / TWO_PI), scalar2=None, op0=ALU.mult,
           r=["angs_c", "angs_s"], w=["kq_f"])
        op("dve", "tensor_copy", out=kq_i, in_=kq_f, r=["kq_f"], w=["kq_i"])
        op("dve", "tensor_copy", out=kq_f, in_=kq_i, r=["kq_i"], w=["kq_f"])
        C1 = 6.28125
        C2 = float(TWO_PI - 6.28125)
        op("dve", "scalar_tensor_tensor", out=red, in0=kq_f, scalar=-C1, in1=angs, op0=ALU.mult, op1=ALU.add,
           r=["kq_f", "angs_c", "angs_s"], w=["red"])
        op("dve", "scalar_tensor_tensor", out=red, in0=kq_f, scalar=-C2, in1=red, op0=ALU.mult, op1=ALU.add,
           r=["kq_f", "red"], w=["red"])
        op("dve", "tensor_scalar", out=wrp, in0=red, scalar1=float(np.pi), scalar2=-TWO_PI, op0=ALU.is_ge, op1=ALU.mult,
           r=["red"], w=["wrp"])
        op("dve", "tensor_tensor", out=red, in0=red, in1=wrp, op=ALU.add, r=["red", "wrp"], w=["red"])
        op("dve", "tensor_scalar", out=wrp, in0=red, scalar1=float(-np.pi), scalar2=TWO_PI, op0=ALU.is_lt, op1=ALU.mult,
           r=["red"], w=["wrp"])
        op("dve", "tensor_tensor", out=red, in0=red, in1=wrp, op=ALU.add, r=["red", "wrp"], w=["red"])
        op("dve", "tensor_scalar", out=red, in0=red, scalar1=float(-np.pi), scalar2=float(np.pi), op0=ALU.max, op1=ALU.min,
           r=["red"], w=["red"])
        op("act", "activation", out=sc_all, in_=red, func=AF.Sin, r=["red"], w=["sc_all"])
        op("dve", "tensor_copy", out=sincos[:, :, 0:8], in_=sc_all[:, 0:32, 0:8], r=["sc_all"], w=["sincos_a"])
        op("dve", "tensor_copy", out=sincos[:, :, 8:16], in_=sc_all[:, 0:32, 12:20], r=["sc_all"], w=["sincos_b"])
        op("dve", "tensor_copy", out=sincos[:, :, 16:20], in_=sc_all[:, 0:32, 8:12], r=["sc_all"], w=["sincos_c"])
        op("dve", "tensor_copy", out=sincos[:, :, 20:24], in_=sc_all[:, 0:32, 20:24], r=["sc_all"], w=["sincos_d"])
        op("dve", "tensor_copy", out=sincosc[:, :, 0:8], in_=sc_all[:, 32:34, 0:8], r=["sc_all"], w=["sincosc_a"])
        op("dve", "tensor_copy", out=sincosc[:, :, 8:16], in_=sc_all[:, 32:34, 12:20], r=["sc_all"], w=["sincosc_b"])
        SINCOS = ["sincos_a", "sincos_b", "sincos_c", "sincos_d"]
        ar.release(m_tmp)
        P.barrier()

        mark('rope_done')
        c_sb = ar.alloc([8], F32)
        cs = ar.alloc([8], F32)
        dma("sp", c_sb, c_d[:, :], w=["c_sb"])
        op("act", "activation", out=cs, in_=c_sb, func=AF.Silu, r=["c_sb"], w=["cs"])

        modbuf = ar.alloc([6, D], F32)
        modflat = flat(modbuf)

        m_layer = ar.mark()

        for l in range(n_layers):
            ar.release(m_layer)
            P.barrier()
            kAT = ar.alloc([2, S], BF16)
            vA1 = ar.alloc([NT, 4, 65], BF16)
            ik4T = ar.alloc([S], BF16)
            ks2T = ar.alloc([S], BF16)
            kw2T = ar.alloc([S], BF16)
            kcvcT = ar.alloc([S], BF16)
            vs1 = ar.alloc([NT, 65], BF16)
            vw1 = ar.alloc([NT, 65], BF16)
            iw_all = ar.alloc([NT, 4], F32)
            gD_all = ar.alloc([NT, 12], F32)
            op("pool", "memset", ap=vA1[:, :, :, 64:65], constant=1.0, w=["vA1_ones"])
            op("pool", "memset", ap=vs1[:, :, 64:65], constant=1.0, w=["vs1_ones"])
            op("pool", "memset", ap=vw1[:, :, 64:65], constant=1.0, w=["vw1_ones"])

            if n_tiles < NT:
                op("pool", "memset", ap=kcvcT, constant=0.0, w=[f"kcvcT_{t_}" for t_ in range(NT)])
            x_src = x_d if l == 0 else xs_d[0]

            m_s0 = ar.mark()
            adaw_buf = [ar.alloc([8, 512], F32) for _ in range(2)]
            adab_buf = [ar.alloc([512], F32) for _ in range(2)]
            gbc = ar.alloc([D], F32)
            csb = ar.alloc([8, 128], F32)
            op("dve", "tensor_copy", out=csb, in_=cs.unsqueeze(2).to_broadcast([128, 8, 128]), r=["cs"], w=["csb"])
            adaw_v = adaw_d[l].rearrange("(kt p) n -> p kt n", p=128)
            for j in range(12):
                bsel = j % 2
                dma("sp", adaw_buf[bsel], adaw_v[:, :, j * 512:(j + 1) * 512], w=[f"adaw{bsel}"])
                dma("sp", adab_buf[bsel], adab_d[l:l + 1, j * 512:(j + 1) * 512].partition_broadcast(128), w=[f"adab{bsel}"])
                for kt in range(8):
                    op("pe", "matmul", out=bk(6 + bsel), lhsT=csb[:, kt, :], rhs=adaw_buf[bsel][:, kt, :], start=(kt == 0), stop=(kt == 7),
                       r=["csb", f"adaw{bsel}"], w=[f"bank{6 + bsel}"])
                op("dve", "tensor_tensor", out=modflat[:, j * 512:(j + 1) * 512], in0=bk(6 + bsel), in1=adab_buf[bsel], op=ALU.add,
                   r=[f"bank{6 + bsel}", f"adab{bsel}"], w=[f"mod{j // 2}"])
            for (slot, gd) in ((1, gmix_d), (4, gffn_d)):
                dma("sp", gbc, gd[l:l + 1, :].partition_broadcast(128), w=["gbc"])
                op("dve", "scalar_tensor_tensor", out=modbuf[:, slot, :], in0=modbuf[:, slot, :], scalar=1.0, in1=gbc,
                   op0=ALU.add, op1=ALU.mult, r=["gbc", f"mod{slot}"], w=[f"mod{slot}"])
            if "mod" in dbg and l == 0:
                dma("sp", dbg["mod"][:, :], modflat, r=[f"mod{s_}" for s_ in range(6)], w=["dbg_mod"])
            ar.release(m_s0)
            P.barrier()

            mark('s0_done')
            m_s1 = ar.mark()
            win_sb = ar.alloc([8, INW], BF16)
            win_v = win_d[l].rearrange("(kt p) n -> p kt n", p=128)
            for kt in range(8):
                dma("pool", win_sb[:, kt, :], win_v[:, kt, :], w=[f"win{kt}"])
            sgug_bc = ar.alloc([256], F32)
            pscale_bc = ar.alloc([256], F32)
            wsT = ar.alloc([4, 128], F32)
            wcT = ar.alloc([4, 128], BF16)
            bsgu = ar.alloc([4], F32)
            wpool_sb = ar.alloc([4, 64], BF16)
            dma("sp", sgug_bc, sgug_d[l:l + 1, :].partition_broadcast(128), w=["sgug_bc"])
            dma("sp", pscale_bc, pscale_d[l:l + 1, :].partition_broadcast(128), w=["pscale_bc"])
            dma("sp", wsT, wsguT_d[l], w=["wsT"])
            dma("sp", bsgu, bsgu_d[l], w=["bsgu"])
            dma("pool", wpool_sb[0:64], wpool_d[l], w=["wpool"])
            op("dve", "tensor_tensor", out=wcT, in0=wsT, in1=trilT.unsqueeze(1).to_broadcast([128, 4, 128]), op=ALU.mult,
               r=["wsT", "trilT"], w=["wcT"])

            xt = [ar.alloc([D], F32) for _ in range(2)]
            hn = ar.alloc([D], F32)
            sqj = hn
            hb = ar.alloc([D], BF16)
            hT = ar.alloc([8, 128], BF16)
            zt = ar.alloc([INW], F32)
            zb = ar.alloc([INW], BF16)
            st = ar.alloc([16], F32)
            rt = ar.alloc([4, 8, 8], F32)
            rtv = ar.alloc([4, 8, 8], F32)
            ikrep = ar.alloc([4, 32], BF16)
            ksrep = ar.alloc([2, 64], BF16)
            kwrep = ar.alloc([2, 64], BF16)
            qpack = [ar.alloc([12, 128], BF16) for _ in range(2)]
            uvg = ar.alloc([512], F32)
            vn = ar.alloc([256], F32)
            vnb = ar.alloc([256], BF16)
            zcb = [ar.alloc([256], BF16) for _ in range(2)]
            pooledT = ar.alloc([4, 128], BF16)
            obc = [ar.alloc([512], BF16) for _ in range(2)]

            print('stage1 arena off', ar.off)
            def load_x(i):
                dma("sp", xt[i % 2], x_src[i * 128:(i + 1) * 128, :], r=[f"xs0_{i}"] if l > 0 else [], w=[f"xt{i % 2}"])

            load_x(0)
            for i in range(n_tiles):
                b = i % 2
                XT = f"xt{b}"
                if i + 1 < n_tiles:
                    load_x(i + 1)
                op("act", "activation", out=sqj, in_=xt[b], func=AF.Square, accum_out=st[:, 0:1], r=[XT], w=["hn", "st0"])
                op("dve", "tensor_scalar", out=st[:, 1:2], in0=st[:, 0:1], scalar1=1.0 / D, scalar2=EPS, op0=ALU.mult, op1=ALU.add,
                   r=["st0"], w=["st1"])
                op("pool", "tensor_tensor", out=st[:, 3:4], in0=st[:, 1:2], in1=negh, op=ALU.pow, r=["st1", "negh"], w=["st3"])
                op("dve", "scalar_tensor_tensor", out=hn, in0=xt[b], scalar=st[:, 3:4], in1=modbuf[:, 1, :], op0=ALU.mult, op1=ALU.mult,
                   r=[XT, "st3", "mod1"], w=["hn"])
                op("dve", "tensor_tensor", out=hb, in0=hn, in1=modbuf[:, 0, :], op=ALU.add, r=["hn", "mod0"], w=["hb"])
                for kt in range(8):
                    op("pe", "transpose", out=bkb(0)[:, kt * 128:(kt + 1) * 128], in_=hb[:, kt * 128:(kt + 1) * 128], identity=ident_b,
                       r=["hb", "ident_b"], w=["bank0"])
                op("act", "copy", out=flat(hT), in_=bkb(0), r=["bank0"], w=["hT"])
                chunks = [(0, 512), (512, 1024), (1024, 1536), (1536, 2048), (2048, INW)]
                for ci, (n0, n1) in enumerate(chunks):
                    pb = 1 + (ci % 2)
                    for kt in range(8):
                        op("pe", "matmul", out=bk(pb)[:, 0:n1 - n0], lhsT=hT[:, kt, :], rhs=win_sb[:, kt, n0:n1], start=(kt == 0), stop=(kt == 7),
                           r=["hT", f"win{kt}"], w=[f"bank{pb}"])
                    if ci % 2 == 0:
                        op("act", "copy", out=zt[:, n0:n1], in_=bk(pb)[:, 0:n1 - n0], r=[f"bank{pb}"], w=[f"zt{ci}"])
                    else:
                        op("dve", "tensor_copy", out=zt[:, n0:n1], in_=bk(pb)[:, 0:n1 - n0], r=[f"bank{pb}"], w=[f"zt{ci}"])
                ZT = [f"zt{ci}" for ci in range(5)]
                if "z" in dbg and l == 0:
                    dma("sp", dbg["z"][i * 128:(i + 1) * 128, :], zt, r=ZT, w=["dbg_z"])

                mark('z_done')
                def rope(view, nh, half, ctab, stab, key, eng, rtb, tg_):
                    x1 = view[:, :, 0:half]
                    x2 = view[:, :, half:2 * half]
                    cb_ = ctab.unsqueeze(1).to_broadcast([128, nh, half])
                    sb_ = stab.unsqueeze(1).to_broadcast([128, nh, half])
                    t1, t2, t3, t4 = (rtb[:, q, 0:nh, 0:half] for q in range(4))
                    op(eng, "tensor_tensor", out=t1, in0=x1, in1=cb_, op=ALU.mult, r=[key] + SINCOS, w=[tg_ + "1"])
                    op(eng, "tensor_tensor", out=t2, in0=x2, in1=sb_, op=ALU.mult, r=[key] + SINCOS, w=[tg_ + "2"])
                    op(eng, "tensor_tensor", out=t3, in0=x2, in1=cb_, op=ALU.mult, r=[key] + SINCOS, w=[tg_ + "3"])
                    op(eng, "tensor_tensor", out=t4, in0=x1, in1=sb_, op=ALU.mult, r=[key] + SINCOS, w=[tg_ + "4"])
                    op(eng, "tensor_tensor", out=x1, in0=t1, in1=t2, op=ALU.subtract, r=[tg_ + "1", tg_ + "2"], w=[key])
                    op(eng, "tensor_tensor", out=x2, in0=t3, in1=t4, op=ALU.add, r=[tg_ + "3", tg_ + "4"], w=[key])

                c64, s64 = sincos[:, i, 0:8], sincos[:, i, 8:16]
                c32, s32 = sincos[:, i, 16:20], sincos[:, i, 20:24]
                rope(zt[:, C_AQ:C_AQ + 512].rearrange("p (h d) -> p h d", d=64), 8, 8, c64, s64, "zt0", "pool", rt, "rtp")
                rope(zt[:, C_IQ:C_IQ + 160].rearrange("p (h d) -> p h d", d=32), 5, 4, c32, s32, "zt1", "dve", rtv, "rtv")
                rope(zt[:, C_DQ:C_DQ + 256].rearrange("p (h d) -> p h d", d=64), 4, 8, c64, s64, "zt3", "dve", rtv, "rtv")
                rope(zt[:, C_KS:C_KS + 256].rearrange("p (h d) -> p h d", d=128), 2, 8, c64, s64, "zt4", "pool", rt, "rtp")
                mark('rope1_done')
                op("act", "copy", out=zb, in_=zt, r=ZT, w=["zb"])
                op("pool", "tensor_copy", out=vA1[:, i, :, 0:64], in_=zb[:, C_AV:C_AV + 256].rearrange("p (h d) -> p h d", d=64),
                   r=["zb"], w=[f"vA1_{i}"])
                op("pool", "tensor_copy", out=vs1[:, i, 0:64], in_=zb[:, C_VS:C_VS + 64], r=["zb"], w=[f"vs1_{i}"])
                op("pool", "tensor_copy", out=vw1[:, i, 0:64], in_=zb[:, C_VW:C_VW + 64], r=["zb"], w=[f"vw1_{i}"])
                op("pool", "tensor_copy", out=iw_all[:, i, :], in_=zt[:, C_IW:C_IW + 4], r=ZT, w=[f"iw_{i}"])
                op("act", "activation", out=gD_all[:, i, :], in_=zt[:, C_DG:C_DG + 12], func=AF.Tanh, scale=0.5, r=ZT, w=[f"gD_{i}"])
                op("pool", "tensor_scalar", out=gD_all[:, i, :], in0=gD_all[:, i, :], scalar1=0.5, scalar2=0.5, op0=ALU.mult, op1=ALU.add,
                   r=[f"gD_{i}"], w=[f"gD_{i}"])
                op("pool", "tensor_copy", out=ikrep, in_=zb[:, C_IK:C_IK + 32].unsqueeze(1).to_broadcast([128, 4, 32]), r=["zb"], w=["ikrep"])
                op("pool", "tensor_copy", out=ksrep, in_=zb[:, C_KS:C_KS + 64].unsqueeze(1).to_broadcast([128, 2, 64]), r=["zb"], w=["ksrep"])
                op("pool", "tensor_copy", out=kwrep, in_=zb[:, C_KW:C_KW + 64].unsqueeze(1).to_broadcast([128, 2, 64]), r=["zb"], w=["kwrep"])
                op("pool", "tensor_copy", out=zcb[b], in_=zb[:, C_CZ:C_CZ + 256], r=["zb"], w=[f"zcb{b}"])
                mark('stash_done')
                tsrc = [(zb[:, C_AQ:C_AQ + 128], "zb"), (zb[:, C_AQ + 128:C_AQ + 256], "zb"),
                        (zb[:, C_AK:C_AK + 128], "zb"), (zb[:, C_AK + 128:C_AK + 256], "zb"),
                        (zb[:, C_IQ:C_IQ + 128], "zb"), (flat(ikrep), "ikrep"),
                        (zb[:, C_DQ:C_DQ + 128], "zb"), (zb[:, C_DQ + 128:C_DQ + 256], "zb"),
                        (flat(ksrep), "ksrep"), (flat(kwrep), "kwrep"),
                        (zb[:, C_KC:C_KC + 128], "zb")]
                for si, (src, key) in enumerate(tsrc):
                    pbank, slot = (3, si) if si < 8 else (4, si - 8)
                    op("pe", "transpose", out=bkb(pbank)[:, slot * 128:(slot + 1) * 128], in_=src, identity=ident_b,
                       r=[key, "ident_b"], w=[f"bank{pbank}"])
                cols = slice(i * 128, (i + 1) * 128)
                qp = qpack[b]
                QP = f"qpack{b}"
                def bc2(src, n):
                    return src.unsqueeze(1).to_broadcast([128, n, 128])
                op("dve", "tensor_tensor", out=qp[:, 0:2, :], in0=bc2(bkb(3)[:, 0:128], 2), in1=hm2, op=ALU.mult, r=["bank3", "hm2"], w=[QP])
                op("dve", "tensor_tensor", out=qp[:, 2:4, :], in0=bc2(bkb(3)[:, 128:256], 2), in1=hm2, op=ALU.mult, r=["bank3", "hm2"], w=[QP])
                op("dve", "tensor_copy", out=kAT[:, :, cols], in_=bkb(3)[:, 256:512].rearrange("p (a b) -> p a b", b=128),
                   r=["bank3"], w=[f"kAT_{i}"])
                op("dve", "tensor_tensor", out=qp[:, 4:8, :], in0=bc2(bkb(3)[:, 512:640], 4), in1=hm4, op=ALU.mult, r=["bank3", "hm4"], w=[QP])
                op("dve", "tensor_copy", out=ik4T[:, cols], in_=bkb(3)[:, 640:768], r=["bank3"], w=[f"ik4T_{i}"])
                op("dve", "tensor_tensor", out=qp[:, 8:10, :], in0=bc2(bkb(3)[:, 768:896], 2), in1=hm2, op=ALU.mult, r=["bank3", "hm2"], w=[QP])
                op("dve", "tensor_tensor", out=qp[:, 10:12, :], in0=bc2(bkb(3)[:, 896:1024], 2), in1=hm2, op=ALU.mult, r=["bank3", "hm2"], w=[QP])
                op("act", "copy", out=ks2T[:, cols], in_=bkb(4)[:, 0:128], r=["bank4"], w=[f"ks2T_{i}"])
                op("act", "copy", out=kw2T[:, cols], in_=bkb(4)[:, 128:256], r=["bank4"], w=[f"kw2T_{i}"])
                op("act", "copy", out=kcvcT[:, cols], in_=bkb(4)[:, 256:384], r=["bank4"], w=[f"kcvcT_{i}"])
                dma("sp", qscr_d[i], flat(qp), r=[QP], w=[f"qscr_{i}"])

                mark('tr_done')
                op("act", "activation", out=uvg, in_=zt[:, C_BU:C_BU + 512], func=AF.Gelu_apprx_tanh, r=ZT, w=["uvg"])
                op("dve", "bn_stats", out=st[:, 4:10], in_=uvg[:, 256:512], r=["uvg"], w=["st4"])
                op("dve", "bn_aggr", out=st[:, 10:12], in_=st[:, 4:10], r=["st4"], w=["st10"])
                op("dve", "tensor_scalar", out=st[:, 12:13], in0=st[:, 11:12], scalar1=EPS, scalar2=None, op0=ALU.add, r=["st10"], w=["st12"])
                op("pool", "tensor_tensor", out=st[:, 14:15], in0=st[:, 12:13], in1=negh, op=ALU.pow, r=["st12", "negh"], w=["st14"])
                op("dve", "tensor_scalar", out=vn, in0=uvg[:, 256:512], scalar1=st[:, 10:11], scalar2=st[:, 14:15],
                   op0=ALU.subtract, op1=ALU.mult, r=["uvg", "st10", "st14"], w=["vn"])
                op("dve", "tensor_tensor", out=vnb, in0=vn, in1=sgug_bc, op=ALU.mult, r=["vn", "sgug_bc"], w=["vnb"])
                for h in range(4):
                    op("pe", "matmul", out=bk(5)[:, h * 64:(h + 1) * 64], lhsT=wcT[:, h, :], rhs=vnb[:, h * 64:(h + 1) * 64],
                       start=True, stop=True, r=["wcT", "vnb"], w=["bank5a"])
                ob = obc[b]
                OB = f"obc{b}"
                for h in range(4):
                    op("dve", "scalar_tensor_tensor", out=ob[:, h * 64:(h + 1) * 64], in0=bk(5)[:, h * 64:(h + 1) * 64],
                       scalar=bsgu[:, h:h + 1], in1=uvg[:, h * 64:(h + 1) * 64], op0=ALU.add, op1=ALU.mult,
                       r=["bank5a", "bsgu", "uvg"], w=[OB + "b"])
                mark('gmlp_done')
                for g in range(4):
                    mm = m0 if i == 0 else mcur
                    op("pe", "matmul", out=bk(6)[0:64, g * 128:(g + 1) * 128], lhsT=zcb[b][:, g * 64:(g + 1) * 64], rhs=mm[:, g, :],
                       start=True, stop=(i == 0), r=[f"zcb{b}", "m0", "mcur"], w=["bank6"])
                    if i > 0:
                        op("pe", "matmul", out=bk(6)[0:64, g * 128:(g + 1) * 128], lhsT=zcb[1 - b][:, g * 64:(g + 1) * 64], rhs=mprev[:, g, :],
                           start=False, stop=True, r=[f"zcb{1 - b}", "mprev"], w=["bank6"])
                op("act", "copy", out=flat(pooledT[0:64]), in_=bk(6)[0:64, :], r=["bank6"], w=["pooledT"])
                for g in range(4):
                    op("pe", "matmul", out=bk(5)[:, 256 + g * 64:256 + (g + 1) * 64], lhsT=pooledT[0:64, g, :], rhs=wpool_sb[0:64, g, :],
                       start=True, stop=True, r=["pooledT", "wpool"], w=["bank5b"])
                op("dve", "tensor_tensor", out=ob[:, 256:512], in0=bk(5)[:, 256:512], in1=pscale_bc, op=ALU.mult,
                   r=["bank5b", "pscale_bc"], w=[OB + "c"])
                dma("sp", cat_d[i][:, 256:768], ob, r=[OB + "b", OB + "c"], w=[f"cat_{i}"])
                if "obc" in dbg and l == 0:
                    dma("sp", dbg["obc"][i * 128:(i + 1) * 128, :], ob, r=[OB + "b", OB + "c"], w=["dbg_obc"])
            ar.release(m_s1)
            P.barrier()

            m_s2 = ar.mark()
            kc2T = ar.alloc([256], BF16)
            vcm1 = ar.alloc([2, 128], BF16)
            m_s1b = ar.mark()
            w1sb = ar.alloc([32, 64], BF16)
            w2sb = ar.alloc([2, 64], BF16)
            peTs = ar.alloc([32], BF16)
            cvec = ar.alloc([2], F32)
            hT2 = ar.alloc([2, 256], BF16)
            kcm = ar.alloc([2, 64], F32)
            kcrep = ar.alloc([2, 2, 64], BF16)
            rtc = ar.alloc([4, 2, 8], F32)
            KCVC = [f"kcvcT_{t_}" for t_ in range(n_tiles)]
            for kv in range(2):
                pr = slice(kv * 64, kv * 64 + 64)
                dma("pool", w1sb[pr], wc1_d[l][kv].rearrange("j d e -> d j e"), w=[f"w1sb{kv}"])
                dma("pool", w2sb[0:64, kv, :], wc2_d[l][kv], w=[f"w2sb{kv}"])
                dma("pool", peTs[pr], peT_d[l][kv], w=[f"peTs{kv}"])
            kv4 = kcvcT.rearrange("p (n r) -> p n r", r=16)
            op("dve", "memset", ap=kcm, constant=0.0, w=["kcm"])
            op("dve", "memset", ap=vcm1, constant=0.0, w=["vcm1"])
            for kv in range(2):
                pr = slice(kv * 64, kv * 64 + 64)
                bank = kv
                for j in range(32):
                    op("pe", "matmul", out=bk(bank)[0:64, 256:257], lhsT=w1sb[pr, j, :], rhs=peTs[pr, j:j + 1], start=(j == 0), stop=(j == 31),
                       r=[f"w1sb{kv}", f"peTs{kv}"], w=[f"bank{bank}"])
                op("dve", "tensor_copy", out=cvec[0:64, kv:kv + 1], in_=bk(bank)[0:64, 256:257], r=[f"bank{bank}"], w=[f"cvec{kv}"])
                for j in range(32):
                    rhs = kv4[pr, 0:255, j] if j < 16 else kv4[pr, 1:256, j - 16]
                    op("pe", "matmul", out=bk(bank)[0:64, 0:255], lhsT=w1sb[pr, j, :], rhs=rhs, start=(j == 0), stop=(j == 31),
                       r=[f"w1sb{kv}"] + KCVC, w=[f"bank{bank}"])
                op("act", "activation", out=hT2[0:64, kv, 0:255], in_=bk(bank)[0:64, 0:255], func=AF.Gelu_apprx_tanh,
                   bias=cvec[0:64, kv:kv + 1], scale=1.0, r=[f"bank{bank}", f"cvec{kv}"], w=[f"hT2_{kv}"])
            for c in range(2):
                size = 128 if c == 0 else 127
                n0 = c * 128
                op("pe", "matmul", out=bk(2)[0:size, c * 64:(c + 1) * 64], lhsT=hT2[0:64, 0, n0:n0 + size], rhs=w2sb[0:64, 0, :],
                   start=True, stop=True, r=["hT2_0", "w2sb0"], w=["bank2"])
                op("pe", "matmul", out=bk(2)[0:size, 128 + c * 64:128 + (c + 1) * 64], lhsT=hT2[0:64, 1, n0:n0 + size], rhs=w2sb[0:64, 1, :],
                   start=True, stop=True, r=["hT2_1", "w2sb1"], w=["bank2"])
                op("dve", "tensor_copy", out=kcm[0:size, c, :], in_=bk(2)[0:size, c * 64:(c + 1) * 64], r=["bank2"], w=["kcm"])
                op("dve", "tensor_copy", out=vcm1[0:size, c, 0:64], in_=bk(2)[0:size, 128 + c * 64:128 + (c + 1) * 64], r=["bank2"], w=["vcm1"])
            op("dve", "tensor_copy", out=vcm1[:, :, 64:128], in_=ovT, r=["ovT"], w=["vcm1"])
            kx1, kx2 = kcm[:, :, 0:8], kcm[:, :, 8:16]
            cc, ss_ = sincosc[:, :, 0:8], sincosc[:, :, 8:16]
            q1, q2, q3, q4 = (rtc[:, q_] for q_ in range(4))
            SCC = ["sincosc_a", "sincosc_b"]
            op("pool", "tensor_tensor", out=q1, in0=kx1, in1=cc, op=ALU.mult, r=["kcm"] + SCC, w=["rtc1"])
            op("pool", "tensor_tensor", out=q2, in0=kx2, in1=ss_, op=ALU.mult, r=["kcm"] + SCC, w=["rtc2"])
            op("pool", "tensor_tensor", out=q3, in0=kx2, in1=cc, op=ALU.mult, r=["kcm"] + SCC, w=["rtc3"])
            op("pool", "tensor_tensor", out=q4, in0=kx1, in1=ss_, op=ALU.mult, r=["kcm"] + SCC, w=["rtc4"])
            op("pool", "tensor_tensor", out=kx1, in0=q1, in1=q2, op=ALU.subtract, r=["rtc1", "rtc2"], w=["kcm"])
            op("pool", "tensor_tensor", out=kx2, in0=q3, in1=q4, op=ALU.add, r=["rtc3", "rtc4"], w=["kcm"])
            op("pool", "tensor_copy", out=kcrep, in_=kcm.unsqueeze(2).to_broadcast([128, 2, 2, 64]), r=["kcm"], w=["kcrep"])
            for c in range(2):
                op("pe", "transpose", out=bkb(3)[:, c * 128:(c + 1) * 128], in_=flat(kcrep[:, c]), identity=ident_b,
                   r=["kcrep", "ident_b"], w=["bank3"])
            op("dve", "tensor_copy", out=kc2T, in_=bkb(3)[:, 0:256], r=["bank3"], w=["kc2T"])
            ar.release(m_s1b)
            P.barrier()
            mark('s1b_done')

            qpk = [ar.alloc([12, 128], BF16) for _ in range(2)]
            score = ar.alloc([S], F32)
            tmpsc = [ar.alloc([512], F32) for _ in range(2)]
            z01 = ar.alloc([S], BF16)
            cz = ar.alloc([S], mybir.dt.float16)
            MBA = [ar.alloc([S], BF16) for _ in range(2)]
            MBD = [ar.alloc([S], BF16) for _ in range(2)]
            PT = [ar.alloc([4, 128], BF16) for _ in range(2)]
            PTc = ar.alloc([4, 128], BF16)
            sst = [ar.alloc([48], F32) for _ in range(2)]
            stp = ar.alloc([32], F32)
            nstp = ar.alloc([32], F32)
            bis = ar.alloc([8], F32)
            imp = ar.alloc([64], F32)
            impm = ar.alloc([64], F32)
            impw = ar.alloc([64], F32)
            m8 = ar.alloc([16], F32)
            selb = ar.alloc([64], BF16)
            bcm = ar.alloc([128], BF16)
            od1 = [ar.alloc([256], F32) for _ in range(2)]
            od2 = ar.alloc([256], F32)
            oa_st = ar.alloc([256], BF16)
            od_st = ar.alloc([256], BF16)
            coef = [ar.alloc([3, 4], F32) for _ in range(2)]
            c300 = ar.alloc([1], F32)
            op("dve", "memset", ap=c300, constant=300.0, w=["c300"])
            NIT = 22
            lcount = [0]

            def attn(i, keys, qslot0, Kf, Vf, vw_, biasf, acc_bank, accw, rk, Lbanks, PTs):
                first = True
                qp_ = qpk[i % 2]
                for n_, k_ in enumerate(keys):
                    lb = lcount[0] % 2
                    lcount[0] += 1
                    Lb = Lbanks[lb]
                    PTb, PTk = PTs[lb]
                    bias = biasf(k_)
                    for h in range(4):
                        outL = bk(Lb)[:, h * 128:(h + 1) * 128]
                        if bias is not None:
                            bap, transposed, bkeys = bias[0], bias[1], bias[2]
                            idm = nident_b if (len(bias) > 3 and bias[3]) else ident_b
                            if transposed:
                                op("pe", "matmul", out=outL, lhsT=idm, rhs=bap, start=True, stop=False, r=["ident_b", "nident_b"] + bkeys, w=[f"bank{Lb}"])
                            else:
                                op("pe", "matmul", out=outL, lhsT=bap, rhs=idm, start=True, stop=False, r=["ident_b", "nident_b"] + bkeys, w=[f"bank{Lb}"])
                        op("pe", "matmul", out=outL, lhsT=Kf(k_, h), rhs=qp_[:, qslot0 + h, :], start=(bias is None), stop=True,
                           r=rk(k_) + [f"qpk{i % 2}"], w=[f"bank{Lb}"])
                    op("act", "activation", out=flat(PTb), in_=bk(Lb), func=AF.Exp, scale=SCALE, r=[f"bank{Lb}"], w=[PTk])
                    for h in range(4):
                        op("pe", "matmul", out=bk(acc_bank)[:, h * accw:h * accw + vw_], lhsT=PTb[:, h, :], rhs=Vf(k_, h),
                           start=first, stop=(n_ == len(keys) - 1), skip_group_check=True,
                           r=[PTk] + rk(k_), w=[f"bank{acc_bank}"])
                        first = False

            def load_q(i):
                dma("sp", flat(qpk[i % 2]), qscr_d[i], r=[f"qscr_{i}"], w=[f"qpk{i % 2}"])

            def search(i):
                p = i % 2
                sp_ = sst[p]
                SS = f"sst{p}_"
                Si = 128 * (i + 1)
                qp_ = qpk[p]
                QK = f"qpk{p}"
                diag = slice(i * 128, (i + 1) * 128)
                nch = (Si + 511) // 512
                for ci in range(nch):
                    c0, c1 = ci * 512, min(Si, ci * 512 + 512)
                    wd_ = c1 - c0
                    rkeys = [f"ik4T_{t_}" for t_ in range(c0 // 128, (c1 + 127) // 128)]
                    for h in range(4):
                        pb = h % 2
                        op("pe", "matmul", out=bk(pb)[:, 0:wd_], lhsT=qp_[:, 4 + h, :], rhs=ik4T[:, c0:c1], start=True, stop=True,
                           r=[QK] + rkeys, w=[f"bank{pb}"])
                        op("act", "activation", out=tmpsc[pb][:, 0:wd_], in_=bk(pb)[:, 0:wd_], func=AF.Relu, r=[f"bank{pb}"], w=[f"tmpsc{pb}"])
                        if h == 0:
                            op("dve", "tensor_scalar", out=score[:, c0:c1], in0=tmpsc[pb][:, 0:wd_], scalar1=iw_all[:, i, 0:1], scalar2=None,
                               op0=ALU.mult, r=[f"tmpsc{pb}", f"iw_{i}"], w=[f"score{ci}"])
                        else:
                            op("dve", "scalar_tensor_tensor", out=score[:, c0:c1], in0=tmpsc[pb][:, 0:wd_], scalar=iw_all[:, i, h:h + 1],
                               in1=score[:, c0:c1], op0=ALU.mult, op1=ALU.add, r=[f"tmpsc{pb}", f"iw_{i}", f"score{ci}"], w=[f"score{ci}"])
                SC = [f"score{ci}" for ci in range(nch)]
                dci = f"score{(i * 128) // 512}"
                sc_ = score[:, 0:Si]
                if i >= 2:
                    op("dve", "tensor_reduce", out=sp_[:, 0:1], in_=sc_, axis=AX.X, op=ALU.min, r=SC, w=[SS + "lo"])
                op("dve", "tensor_tensor", out=score[:, diag], in0=score[:, diag], in1=cb_b, op=ALU.add,
                   r=[dci, "cb_b"] + ([SS + "lo"] if i >= 2 else []), w=[dci])
                if i >= 2:
                    op("dve", "tensor_reduce", out=sp_[:, 1:2], in_=sc_, axis=AX.X, op=ALU.max, r=SC, w=[SS + "hi"])
                    op("dve", "tensor_tensor", out=sp_[:, 2:3], in0=sp_[:, 1:2], in1=sp_[:, 0:1], op=ALU.subtract, r=[SS + "lo", SS + "hi"], w=[SS + "rng"])
                    op("dve", "tensor_scalar", out=stp[:, 0:NIT], in0=pow2[:, 0:NIT], scalar1=sp_[:, 2:3], scalar2=None, op0=ALU.mult,
                       r=[SS + "rng", "pow2"], w=["stp"])
                    op("dve", "tensor_scalar", out=nstp[:, 0:NIT], in0=stp[:, 0:NIT], scalar1=-1.0, scalar2=None, op0=ALU.mult, r=["stp"], w=["nstp"])
                    op("dve", "scalar_tensor_tensor", out=bis[:, 0:1], in0=sp_[:, 0:1], scalar=-1.0, in1=nstp[:, 0:1], op0=ALU.mult, op1=ALU.add,
                       r=[SS + "lo", "nstp"], w=["b_nT"])
                ncc = 2 if i >= 16 else 1
                cmp_bias = {}
                for c in range(ncc):
                    thr_ic = 2048.0 * c + 31.0 - 128.0 * i
                    if thr_ic > -2032.0:
                        cmp_bias[c] = thr_ic

                def cmp_biasf(c):
                    if c not in cmp_bias:
                        return None
                    op("pool", "tensor_scalar", out=bcm, in0=d0, scalar1=float(cmp_bias[c]), scalar2=NEGB, op0=ALU.is_lt, op1=ALU.mult,
                       r=["d0"], w=["bcm"])
                    return (bcm, True, ["bcm"])
                attn(i, list(range(ncc)), 8, lambda c, h: kc2T[:, c * 128:(c + 1) * 128], lambda c, h: vcm1[:, c, :], 128,
                     cmp_biasf, 5, 128, lambda c: ["kc2T", "vcm1"], [0, 1], [(PTc, "PTc"), (PTc, "PTc")])
                if i >= 2:
                    a_ = max(64, int(Si * 0.45) // 64 * 64)
                    nA = Si - a_
                    op("act", "activation", out=z01[:, 0:Si], in_=sc_, func=AF.Sign, bias=-1.0e-30, scale=1.0, accum_out=sp_[:, 40:41],
                       r=SC, w=["z01", SS + "sump"])
                    op("act", "activation", out=z01[:, 0:Si], in_=sc_, func=AF.Sign, bias=1.0e-30, scale=1.0, accum_out=sp_[:, 41:42],
                       r=SC, w=["z01", SS + "sumn"])
                    op("dve", "tensor_tensor", out=sp_[:, 3:4], in0=sp_[:, 0:1], in1=stp[:, 0:1], op=ALU.add, r=[SS + "lo", "stp"], w=[SS + "mid"])
                    for k_ in range(NIT):
                        op("act", "activation", out=z01[:, a_:Si], in_=score[:, a_:Si], func=AF.Sign, bias=sp_[:, 3:4], scale=-1.0, accum_out=sp_[:, 42:43],
                           r=SC + [SS + "mid"], w=["z01", SS + "sga"])
                        op("dve", "tensor_scalar", out=cz[:, 0:a_], in0=score[:, 0:a_], scalar1=sp_[:, 3:4], scalar2=0.0, op0=ALU.is_ge, op1=ALU.add,
                           accum_out=sp_[:, 4:5], r=SC + [SS + "mid"], w=["cz", SS + "cnt"])
                        op("act", "activation", out=sp_[:, 43:44], in_=sp_[:, 42:43], func=AF.Identity, scale=0.5, bias=float(255.5 - nA / 2.0),
                           r=[SS + "sga"], w=[SS + "tot"])
                        op("dve", "tensor_scalar", out=sp_[:, 5:6], in0=sp_[:, 4:5], scalar1=sp_[:, 43:44], scalar2=stp[:, k_:k_ + 1],
                           op0=ALU.is_ge, op1=ALU.mult, r=[SS + "cnt", SS + "tot", "stp"], w=[SS + "delta"])
                        if k_ < NIT - 1:
                            op("dve", "scalar_tensor_tensor", out=sp_[:, 3:4], in0=sp_[:, 3:4], scalar=stp[:, k_ + 1:k_ + 2], in1=sp_[:, 5:6],
                               op0=ALU.subtract, op1=ALU.add, r=[SS + "mid", "stp", SS + "delta"], w=[SS + "mid"])
                        else:
                            op("dve", "scalar_tensor_tensor", out=sp_[:, 6:7], in0=sp_[:, 3:4], scalar=stp[:, k_:k_ + 1], in1=sp_[:, 5:6],
                               op0=ALU.subtract, op1=ALU.add, r=[SS + "mid", "stp", SS + "delta"], w=[SS + "thr"])
                MA = f"MBA{p}"
                if i >= 2:
                    op("dve", "tensor_scalar", out=sp_[:, 32:33], in0=sp_[:, 40:41], scalar1=float(Si), scalar2=0.5, op0=ALU.add, op1=ALU.mult,
                       r=[SS + "sump"], w=[SS + "cpos"])
                    op("dve", "tensor_scalar", out=sp_[:, 33:34], in0=sp_[:, 41:42], scalar1=float(Si), scalar2=0.5, op0=ALU.add, op1=ALU.mult,
                       r=[SS + "sumn"], w=[SS + "cnn"])
                    op("dve", "tensor_scalar", out=sp_[:, 34:35], in0=sp_[:, 32:33], scalar1=255.5, scalar2=None, op0=ALU.is_lt, r=[SS + "cpos"], w=[SS + "tfa"])
                    op("dve", "scalar_tensor_tensor", out=sp_[:, 35:36], in0=sp_[:, 33:34], scalar=255.5, in1=sp_[:, 34:35], op0=ALU.is_gt, op1=ALU.mult,
                       r=[SS + "cnn", SS + "tfa"], w=[SS + "tf"])
                    op("dve", "tensor_scalar", out=sp_[:, 36:37], in0=sp_[:, 32:33], scalar1=-1.0, scalar2=256.0, op0=ALU.mult, op1=ALU.add,
                       r=[SS + "cpos"], w=[SS + "need0"])
                    op("dve", "tensor_tensor", out=sp_[:, 37:38], in0=sp_[:, 36:37], in1=sp_[:, 35:36], op=ALU.mult, r=[SS + "need0", SS + "tf"], w=[SS + "need"])
                    op("dve", "tensor_scalar", out=sp_[:, 38:39], in0=sp_[:, 35:36], scalar1=-1.0, scalar2=1.0, op0=ALU.mult, op1=ALU.add,
                       r=[SS + "tf"], w=[SS + "ntf"])
                    op("dve", "tensor_tensor", out=sp_[:, 6:7], in0=sp_[:, 6:7], in1=sp_[:, 38:39], op=ALU.mult, r=[SS + "thr", SS + "ntf"], w=[SS + "thr"])
                    op("dve", "scalar_tensor_tensor", out=sp_[:, 6:7], in0=sp_[:, 35:36], scalar=1.0e-30, in1=sp_[:, 6:7], op0=ALU.mult, op1=ALU.add,
                       r=[SS + "tf", SS + "thr"], w=[SS + "thr"])
                    op("dve", "tensor_scalar", out=z01[:, 0:Si], in0=sc_, scalar1=0.0, scalar2=None, op0=ALU.is_equal, r=SC, w=["z01"])
                    op("dve", "tensor_tensor_scan", out=cz[:, 0:Si], data0=z01[:, 0:Si], data1=c300[:, 0:1].to_broadcast([128, Si]), initial=0.0,
                       op0=ALU.add, op1=ALU.min, r=["z01", "c300"], w=["cz"])
                    op("dve", "scalar_tensor_tensor", out=z01[:, 0:Si], in0=cz[:, 0:Si], scalar=sp_[:, 37:38], in1=z01[:, 0:Si], op0=ALU.is_le, op1=ALU.mult,
                       r=["cz", SS + "need", "z01"], w=["z01"])
                    op("dve", "scalar_tensor_tensor", out=sc_, in0=z01[:, 0:Si], scalar=1.0e-20, in1=sc_, op0=ALU.mult, op1=ALU.add,
                       r=["z01"] + SC, w=SC)
                else:
                    op("dve", "memset", ap=sp_[:, 6:7], constant=-1.0e4, w=[SS + "thr"])
                op("dve", "tensor_scalar", out=sp_[:, 44:45], in0=sp_[:, 6:7], scalar1=1.0e33, scalar2=None, op0=ALU.mult, r=[SS + "thr"], w=[SS + "thrL"])
                op("act", "activation", out=MBA[p][:, 0:Si], in_=sc_, func=AF.Relu, bias=sp_[:, 44:45], scale=-1.0e33, r=SC + [SS + "thrL"], w=[MA])
                pcm = bk(5).rearrange("p (h w) -> p h w", w=128)
                op("dve", "tensor_reduce", out=sp_[:, 8:12], in_=pcm[:, :, 64:128], axis=AX.X, op=ALU.add, r=["bank5"], w=[SS + "zc"])
                op("dve", "tensor_scalar", out=sp_[:, 8:12], in0=sp_[:, 8:12], scalar1=1.0e-30, scalar2=None, op0=ALU.max, r=[SS + "zc"], w=[SS + "zc"])
                op("dve", "reciprocal", out=sp_[:, 12:16], in_=sp_[:, 8:12], r=[SS + "zc"], w=[SS + "rzc"])
                op("dve", "tensor_scalar", out=imp, in0=pcm[:, 0, 64:128], scalar1=sp_[:, 12:13], scalar2=None, op0=ALU.mult, r=["bank5", SS + "rzc"], w=["imp"])
                for h in range(1, 4):
                    op("dve", "scalar_tensor_tensor", out=imp, in0=pcm[:, h, 64:128], scalar=sp_[:, 12 + h:13 + h], in1=imp, op0=ALU.mult, op1=ALU.add,
                       r=["bank5", SS + "rzc", "imp"], w=["imp"])
                gv = gD_all[:, i, :].rearrange("p (h g) -> p h g", g=3)

                def bc4(ap):
                    return ap.unsqueeze(2).to_broadcast([128, 4, 64])
                op("dve", "tensor_tensor", out=coef[p][:, 0, :], in0=gv[:, :, 0], in1=sp_[:, 12:16], op=ALU.mult, r=[f"gD_{i}", SS + "rzc"], w=[f"coef0_{p}"])
                op("dve", "tensor_tensor", out=od1[p].rearrange("p (h d) -> p h d", d=64), in0=pcm[:, :, 0:64], in1=bc4(coef[p][:, 0, :]), op=ALU.mult,
                   r=["bank5", f"coef0_{p}"], w=[f"od1_{p}"])
                op("dve", "tensor_copy", out=impm, in_=imp, r=["imp"], w=["impm"])
                op("dve", "memset", ap=impm[:, 0:1], constant=BIG, r=["impm"], w=["impm"])
                if 2 * i + 2 < 64:
                    op("dve", "memset", ap=impm[:, 2 * i + 2:64], constant=-BIG, r=["impm"], w=["impm"])
                op("dve", "memset", ap=impm[0:64, 2 * i:2 * i + 1], constant=BIG, r=["impm"], w=["impm"])
                op("dve", "memset", ap=impm[0:64, 2 * i + 1:2 * i + 2], constant=-BIG, r=["impm"], w=["impm"])
                op("dve", "memset", ap=impm[64:128, 2 * i + 1:2 * i + 2], constant=BIG, r=["impm"], w=["impm"])
                op("dve", "max", out=m8[:, 0:8], in_=impm, r=["impm"], w=["m8a"])
                op("dve", "match_replace", out=impw, in_to_replace=m8[:, 0:8], in_values=impm, imm_value=-BIG, r=["impm", "m8a"], w=["impw"])
                op("dve", "max", out=m8[:, 8:16], in_=impw, r=["impw"], w=["m8b"])
                op("dve", "tensor_scalar", out=sp_[:, 16:17], in0=m8[:, 15:16], scalar1=-1.0e29, scalar2=None, op0=ALU.max, r=["m8b"], w=[SS + "t16"])
                op("dve", "tensor_scalar", out=selb, in0=impm, scalar1=sp_[:, 16:17], scalar2=NEGB, op0=ALU.is_lt, op1=ALU.mult,
                   r=["impm", SS + "t16"], w=["selb"])
                nb_ = 2 * (i + 1)
                MD = f"MBD{p}"
                op("dve", "tensor_copy", out=MBD[p][:, 0:Si].rearrange("p (j r) -> p j r", r=64),
                   in_=selb[:, 0:nb_].unsqueeze(2).to_broadcast([128, nb_, 64]), r=["selb"], w=[MD])
                op("dve", "tensor_tensor", out=MBD[p][:, diag], in0=MBD[p][:, diag], in1=cb_b, op=ALU.add, r=[MD, "cb_b"], w=[MD])
                if "mba" in dbg and l == 0 and i == n_tiles - 1:
                    dma("sp", dbg["mba"][:, :], MBA[p], r=[MA], w=["dbg_mba"])

            def attend(i):
                p = i % 2
                sp_ = sst[p]
                SS = f"sst{p}_"
                KT = list(range(i + 1))
                LB = [2, 3]
                PTS = [(PT[0], "PT0"), (PT[1], "PT1")]
                attn(i, KT, 0, lambda k_, h: kAT[:, h // 2, k_ * 128:(k_ + 1) * 128], lambda k_, h: vA1[:, k_, h, :], 65,
                     lambda k_: (MBA[p][:, k_ * 128:(k_ + 1) * 128], False, [f"MBA{p}"], True), 4, 65,
                     lambda k_: [f"kAT_{k_}", f"vA1_{k_}", "vA1_ones"], LB, PTS)
                WK = list(range(max(0, i - 4), i + 1))

                def win_biasf(k_):
                    if k_ == i:
                        return (cb_b, False, ["cb_b"])
                    if k_ == i - 4:
                        return (wb_b, False, ["wb_b"])
                    return None
                attn(i, WK, 8, lambda k_, h: kw2T[:, k_ * 128:(k_ + 1) * 128], lambda k_, h: vw1[:, k_, :], 65,
                     win_biasf, 7, 65, lambda k_: [f"kw2T_{k_}", f"vw1_{k_}", "vw1_ones"], LB, PTS)
                attn(i, KT, 8, lambda k_, h: ks2T[:, k_ * 128:(k_ + 1) * 128], lambda k_, h: vs1[:, k_, :], 65,
                     lambda k_: (MBD[p][:, k_ * 128:(k_ + 1) * 128], False, [f"MBD{p}"]), 6, 65,
                     lambda k_: [f"ks2T_{k_}", f"vs1_{k_}", "vs1_ones"], LB, PTS)
                pA = bk(4)[:, 0:260].rearrange("p (h w) -> p h w", w=65)
                pS = bk(6)[:, 0:260].rearrange("p (h w) -> p h w", w=65)
                pW = bk(7)[:, 0:260].rearrange("p (h w) -> p h w", w=65)

                def bc4(ap):
                    return ap.unsqueeze(2).to_broadcast([128, 4, 64])
                op("dve", "reciprocal", out=sp_[:, 20:24], in_=pA[:, :, 64], r=["bank4"], w=[SS + "rza"])
                op("dve", "tensor_tensor", out=oa_st.rearrange("p (h d) -> p h d", d=64), in0=pA[:, :, 0:64], in1=bc4(sp_[:, 20:24]),
                   op=ALU.mult, r=["bank4", SS + "rza"], w=["oa_st"])
                dma("sp", cat_d[i][:, 0:256], oa_st, r=["oa_st"], w=[f"cata_{i}"])
                op("dve", "reciprocal", out=sp_[:, 24:28], in_=pS[:, :, 64], r=["bank6"], w=[SS + "rzs"])
                op("dve", "reciprocal", out=sp_[:, 28:32], in_=pW[:, :, 64], r=["bank7"], w=[SS + "rzw"])
                gv = gD_all[:, i, :].rearrange("p (h g) -> p h g", g=3)
                op("dve", "tensor_tensor", out=coef[p][:, 1, :], in0=gv[:, :, 1], in1=sp_[:, 24:28], op=ALU.mult, r=[f"gD_{i}", SS + "rzs"], w=[f"coef1_{p}"])
                op("dve", "tensor_tensor", out=coef[p][:, 2, :], in0=gv[:, :, 2], in1=sp_[:, 28:32], op=ALU.mult, r=[f"gD_{i}", SS + "rzw"], w=[f"coef2_{p}"])
                o2 = od2.rearrange("p (h d) -> p h d", d=64)
                op("dve", "tensor_tensor", out=o2, in0=pS[:, :, 0:64], in1=bc4(coef[p][:, 1, :]), op=ALU.mult, r=["bank6", f"coef1_{p}"], w=["od2"])
                op("pool", "tensor_tensor", out=od1[p], in0=od1[p], in1=od2, op=ALU.add, r=[f"od1_{p}", "od2"], w=[f"od1_{p}"])
                op("dve", "tensor_tensor", out=o2, in0=pW[:, :, 0:64], in1=bc4(coef[p][:, 2, :]), op=ALU.mult, r=["bank7", f"coef2_{p}"], w=["od2"])
                op("pool", "tensor_tensor", out=od_st, in0=od1[p], in1=od2, op=ALU.add, r=[f"od1_{p}", "od2"], w=["od_st"])
                dma("sp", cat_d[i][:, 768:1024], od_st, r=["od_st"], w=[f"catd_{i}"])

            print('stage2 arena off', ar.off)
            load_q(0)
            search(0)
            for i in range(n_tiles):
                if i + 1 < n_tiles:
                    load_q(i + 1)
                    P.capture()
                    search(i + 1)
                    ys = P.end_capture()
                    P.capture()
                    attend(i)
                    xs_ = P.end_capture()
                    P.emit_merged([xs_, ys])
                else:
                    attend(i)
            mark('s2_done')
            ar.release(m_layer)
            P.barrier()

            h2T = ar.alloc([8, S], BF16)
            gateT = ar.alloc([S], BF16)
            sel16 = ar.alloc([16, 128], BF16)
            gfin_bc = ar.alloc([D], F32)
            dma("pool", sel16[0:16], kd["k_sel16"][:, :, :], w=["sel16"])
            dma("sp", gfin_bc, gfin_d[0:1, :].partition_broadcast(128), w=["gfin_bc"])
            m_s3 = ar.mark()
            rw32 = ar.alloc([8, 16], F32)
            rb_bc = ar.alloc([16], F32)
            dma("sp", rw32, rw_d.rearrange("(kt p) e -> p kt e", p=128), w=["rw32"])
            dma("sp", rb_bc, rb_d[0:1, :].partition_broadcast(128), w=["rb_bc"])

            wout_sb = ar.alloc([8, D], BF16)
            wout_v = wout_d[l].rearrange("(kt p) n -> p kt n", p=128)
            for kt in range(8):
                dma("pool", wout_sb[:, kt, :], wout_v[:, kt, :], w=[f"wout{kt}"])
            catl = [ar.alloc([D], BF16) for _ in range(4)]
            catT = [ar.alloc([8, 128], BF16) for _ in range(2)]
            xin = [ar.alloc([D], F32) for _ in range(4)]
            xa = [ar.alloc([D], F32) for _ in range(4)]
            h2 = [ar.alloc([D], F32) for _ in range(2)]
            h2b = [ar.alloc([D], BF16) for _ in range(2)]
            h2T32 = [ar.alloc([8, 128], F32) for _ in range(2)]
            rs2 = [ar.alloc([128], F32) for _ in range(2)]
            gateb2 = [ar.alloc([16], BF16) for _ in range(2)]
            print('stage3a arena off', ar.off)

            def load_xa(i):
                q = i % 4
                dma("sp", catl[q], cat_d[i], r=[f"cat_{i}", f"cata_{i}", f"catd_{i}"], w=[f"catl{q}"])
                dma("sp", xin[q], x_src[i * 128:(i + 1) * 128, :], r=[f"xs0_{i}"] if l > 0 else [], w=[f"xin{q}"])

            def body3a(i):
                b = i % 2
                q = i % 4
                B0 = 4 * b
                XA = f"xa{q}"
                sfx = f"_{b}"
                rs_ = rs2[b]
                gateb = gateb2[b]
                cols = slice(i * 128, (i + 1) * 128)
                for kt in range(8):
                    op("pe", "transpose", out=bkb(B0)[:, kt * 128:(kt + 1) * 128], in_=catl[q][:, kt * 128:(kt + 1) * 128], identity=ident_b,
                       r=[f"catl{q}", "ident_b"], w=[f"bank{B0}"])
                op("act", "copy", out=flat(catT[b]), in_=bkb(B0), r=[f"bank{B0}"], w=["catT" + sfx])
                for nc_ in range(2):
                    pb = B0 + 1 + nc_
                    for kt in range(8):
                        op("pe", "matmul", out=bk(pb), lhsT=catT[b][:, kt, :], rhs=wout_sb[:, kt, nc_ * 512:(nc_ + 1) * 512],
                           start=(kt == 0), stop=(kt == 7), r=["catT" + sfx, f"wout{kt}"], w=[f"bank{pb}"])
                    op("dve", "tensor_tensor", out=xa[q][:, nc_ * 512:(nc_ + 1) * 512], in0=bk(pb), in1=modbuf[:, 2, nc_ * 512:(nc_ + 1) * 512],
                       op=ALU.mult, r=[f"bank{pb}", "mod2"], w=[XA])
                op("pool", "tensor_tensor", out=xa[q], in0=xa[q], in1=xin[q], op=ALU.add, r=[XA, f"xin{q}"], w=[XA])
                dma("sp", xs_d[1][i * 128:(i + 1) * 128, :], xa[q], r=[XA], w=[f"xs1_{i}"])
                if "xmid" in dbg and l == 0:
                    dma("sp", dbg["xmid"][i * 128:(i + 1) * 128, :], xa[q], r=[XA], w=["dbg_xmid"])
                H2 = "h2" + sfx
                op("act", "activation", out=h2[b], in_=xa[q], func=AF.Square, accum_out=rs_[:, 0:1], r=[XA], w=[H2, "r_ss" + sfx])
                op("dve", "tensor_scalar", out=rs_[:, 1:2], in0=rs_[:, 0:1], scalar1=1.0 / D, scalar2=EPS, op0=ALU.mult, op1=ALU.add, r=["r_ss" + sfx], w=["r_ms" + sfx])
                op("pool", "tensor_tensor", out=rs_[:, 3:4], in0=rs_[:, 1:2], in1=negh, op=ALU.pow, r=["r_ms" + sfx, "negh"], w=["r_rstd" + sfx])
                op("dve", "scalar_tensor_tensor", out=h2[b], in0=xa[q], scalar=rs_[:, 3:4], in1=modbuf[:, 4, :], op0=ALU.mult, op1=ALU.mult,
                   r=[XA, "r_rstd" + sfx, "mod4"], w=[H2])
                op("dve", "tensor_tensor", out=h2[b], in0=h2[b], in1=modbuf[:, 3, :], op=ALU.add, r=[H2, "mod3"], w=[H2])
                op("act", "copy", out=h2b[b], in_=h2[b], r=[H2], w=["h2b" + sfx])
                for kt in range(8):
                    op("pe", "transpose", out=bkb(B0)[:, kt * 128:(kt + 1) * 128], in_=h2b[b][:, kt * 128:(kt + 1) * 128], identity=ident_b,
                       r=["h2b" + sfx, "ident_b"], w=[f"bank{B0}"])
                op("act", "copy", out=h2T[:, :, cols], in_=bkb(B0).rearrange("p (a b) -> p a b", b=128), r=[f"bank{B0}"], w=[f"h2T_{i}"])
                for kt in range(8):
                    pb = B0 + 1 + kt // 4
                    op("pe", "transpose", out=bk(pb)[:, (kt % 4) * 128:(kt % 4 + 1) * 128], in_=h2[b][:, kt * 128:(kt + 1) * 128], identity=ident_f,
                       r=[H2, "ident_f"], w=[f"bank{pb}"])
                op("dve", "tensor_copy", out=flat(h2T32[b][:, 0:4, :]), in_=bk(B0 + 1), r=[f"bank{B0 + 1}"], w=["h2T32a" + sfx])
                op("dve", "tensor_copy", out=flat(h2T32[b][:, 4:8, :]), in_=bk(B0 + 2), r=[f"bank{B0 + 2}"], w=["h2T32b" + sfx])
                RB = B0 + 3
                for kt in range(8):
                    op("pe", "matmul", out=bk(RB)[:, 0:16], lhsT=h2T32[b][:, kt, :], rhs=rw32[:, kt, :], start=(kt == 0), stop=(kt == 7),
                       r=["h2T32a" + sfx, "h2T32b" + sfx, "rw32"], w=[f"bank{RB}"])
                aff = rs_[:, 16:32]
                bia = rs_[:, 32:48]
                bv = bia.rearrange("p (g e) -> p g e", e=4)
                eq = rs_[:, 48:64]
                eqv = eq.rearrange("p (g e) -> p g e", e=4)
                mb = rs_[:, 64:80]
                mbv = mb.rearrange("p (g e) -> p g e", e=4)
                K_ = lambda nm: nm + sfx
                op("act", "activation", out=aff, in_=bk(RB)[:, 0:16], func=AF.Sigmoid, r=[f"bank{RB}"], w=[K_("r_aff")])
                op("dve", "tensor_tensor", out=bia, in0=aff, in1=rb_bc, op=ALU.add, r=[K_("r_aff"), "rb_bc"], w=[K_("r_bia")])
                op("dve", "tensor_reduce", out=rs_[:, 4:8], in_=bv, axis=AX.X, op=ALU.max, r=[K_("r_bia")], w=[K_("r_m1")])
                op("dve", "tensor_tensor", out=eqv, in0=bv, in1=rs_[:, 4:8].unsqueeze(2).to_broadcast([128, 4, 4]), op=ALU.is_equal,
                   r=[K_("r_bia"), K_("r_m1")], w=[K_("r_eq")])
                op("dve", "scalar_tensor_tensor", out=eq, in0=eq, scalar=-BIG, in1=bia, op0=ALU.mult, op1=ALU.add, r=[K_("r_eq"), K_("r_bia")], w=[K_("r_eq")])
                op("dve", "tensor_reduce", out=rs_[:, 8:12], in_=eqv, axis=AX.X, op=ALU.max, r=[K_("r_eq")], w=[K_("r_m2")])
                op("dve", "tensor_tensor", out=rs_[:, 8:12], in0=rs_[:, 8:12], in1=rs_[:, 4:8], op=ALU.add, r=[K_("r_m1"), K_("r_m2")], w=[K_("r_gs")])
                op("dve", "tensor_reduce", out=rs_[:, 12:13], in_=rs_[:, 8:12], axis=AX.X, op=ALU.max, r=[K_("r_gs")], w=[K_("r_gmax")])
                op("dve", "tensor_scalar", out=rs_[:, 8:12], in0=rs_[:, 8:12], scalar1=rs_[:, 12:13], scalar2=-BIG, op0=ALU.is_lt, op1=ALU.mult,
                   r=[K_("r_gs"), K_("r_gmax")], w=[K_("r_pen")])
                op("dve", "tensor_tensor", out=mbv, in0=bv, in1=rs_[:, 8:12].unsqueeze(2).to_broadcast([128, 4, 4]), op=ALU.add,
                   r=[K_("r_bia"), K_("r_pen")], w=[K_("r_mb")])
                op("dve", "max", out=rs_[:, 80:88], in_=mb, r=[K_("r_mb")], w=[K_("r_m8")])
                op("dve", "tensor_scalar", out=eq, in0=mb, scalar1=rs_[:, 81:82], scalar2=None, op0=ALU.is_ge, r=[K_("r_mb"), K_("r_m8"), K_("r_eq")], w=[K_("r_sel")])
                op("dve", "tensor_tensor", out=eq, in0=eq, in1=aff, op=ALU.mult, r=[K_("r_sel"), K_("r_aff")], w=[K_("r_ta")])
                op("dve", "tensor_reduce", out=rs_[:, 13:14], in_=eq, axis=AX.X, op=ALU.add, r=[K_("r_ta")], w=[K_("r_ws")])
                op("dve", "reciprocal", out=rs_[:, 14:15], in_=rs_[:, 13:14], r=[K_("r_ws")], w=[K_("r_rws")])
                op("dve", "tensor_scalar", out=gateb, in0=eq, scalar1=rs_[:, 14:15], scalar2=None, op0=ALU.mult, r=[K_("r_ta"), K_("r_rws")], w=[K_("gateb")])
                op("pe", "transpose", out=bkb(RB)[0:16, 512:640], in_=gateb, identity=ident_b, r=[K_("gateb"), "ident_b"], w=[f"bank{RB}"])
                op("dve", "tensor_copy", out=gateT[0:16, cols], in_=bkb(RB)[0:16, 512:640], r=[f"bank{RB}"], w=[f"gateT_{i}"])
                if "gate" in dbg and l == 0:
                    dma("sp", dbg["gate"][i * 128:(i + 1) * 128, :], gateb, r=[K_("gateb")], w=["dbg_gate"])

            load_xa(0)
            if n_tiles > 1:
                load_xa(1)
            for i in range(0, n_tiles, 2):
                for t_ in (i + 2, i + 3):
                    if t_ < n_tiles:
                        load_xa(t_)
                P.capture()
                body3a(i)
                s_a = P.end_capture()
                if i + 1 < n_tiles:
                    P.capture()
                    body3a(i + 1)
                    s_b = P.end_capture()
                    P.emit_merged([s_a, s_b])
                else:
                    P.emit_merged([s_a])
            ar.release(m_s3)
            P.barrier()
            mark('s3a_done')

            NSLOT = 6
            wg_sb = [ar.alloc([8, 256], BF16) for _ in range(NSLOT)]
            wu_sb = [ar.alloc([8, 256], BF16) for _ in range(NSLOT)]
            wd_sb = [ar.alloc([2, D], BF16) for _ in range(NSLOT)]
            sg = [ar.alloc([256], F32) for _ in range(2)]
            hu = [ar.alloc([256], F32) for _ in range(2)]
            hid = [ar.alloc([256], BF16) for _ in range(2)]
            xacc = [ar.alloc([D], F32) for _ in range(2)]
            xtmp = ar.alloc([D], F32)
            fst = ar.alloc([8], F32)
            print('stage3b arena off', ar.off)
            TG = 256
            n_tg = (n_tiles * 128) // TG

            def load_expert(e):
                sl = e % NSLOT
                dma("pool", wg_sb[sl], wg_d[l][e].rearrange("(kt p) f -> p kt f", p=128), w=[f"wg{sl}"])
                dma("pool", wu_sb[sl], wu_d[l][e].rearrange("(kt p) f -> p kt f", p=128), w=[f"wu{sl}"])
                dma("pool", wd_sb[sl], wd_d[l][e].rearrange("(ft p) d -> p ft d", p=128), w=[f"wd{sl}"])

            for e in range(min(NSLOT, 16)):
                load_expert(e)
            last_layer = (l == n_layers - 1)
            for G in range(4):
                for tg in range(n_tg):
                    tcols = slice(tg * TG, (tg + 1) * TG)
                    HK = [f"h2T_{tg * 2}", f"h2T_{tg * 2 + 1}"]
                    GK = [f"gateT_{tg * 2}", f"gateT_{tg * 2 + 1}"]
                    for t2 in range(2):
                        ti = tg * 2 + t2
                        dma("sp", xacc[t2], xs_d[1][ti * 128:(ti + 1) * 128, :], r=[f"xs1_{ti}"], w=[f"xacc{t2}"])
                    for ei in range(4):
                        e = G * 4 + ei
                        sl = e % NSLOT
                        for ft in range(2):
                            pb = 4 + ft
                            for kt in range(8):
                                op("pe", "matmul", out=bk(pb)[:, 0:256], lhsT=wg_sb[sl][:, kt, ft * 128:(ft + 1) * 128], rhs=h2T[:, kt, tcols],
                                   start=(kt == 0), stop=(kt == 7), r=[f"wg{sl}"] + HK, w=[f"bank{pb}"])
                            for kt in range(8):
                                op("pe", "matmul", out=bk(pb)[:, 256:512], lhsT=wu_sb[sl][:, kt, ft * 128:(ft + 1) * 128], rhs=h2T[:, kt, tcols],
                                   start=(kt == 0), stop=(kt == 7), skip_group_check=True, r=[f"wu{sl}"] + HK, w=[f"bank{pb}"])
                        op("pe", "matmul", out=bk(6)[:, 0:256], lhsT=sel16[0:16, e, :], rhs=gateT[0:16, tcols], start=True, stop=True,
                           r=["sel16"] + GK, w=["bank6"])
                        for ft in range(2):
                            pb = 4 + ft
                            op("act", "activation", out=sg[ft], in_=bk(pb)[:, 0:256], func=AF.Silu, r=[f"bank{pb}"], w=[f"sg{ft}"])
                            op("dve", "tensor_tensor", out=hu[ft], in0=sg[ft], in1=bk(pb)[:, 256:512], op=ALU.mult, r=[f"sg{ft}", f"bank{pb}"], w=[f"hu{ft}"])
                            op("dve", "tensor_tensor", out=hid[ft], in0=hu[ft], in1=bk(6)[:, 0:256], op=ALU.mult, r=[f"hu{ft}", "bank6"], w=[f"hid{ft}"])
                        for ft in range(2):
                            for t2 in range(2):
                                for dc in range(2):
                                    pob = t2 * 2 + dc
                                    op("pe", "matmul", out=bk(pob), lhsT=hid[ft][:, t2 * 128:(t2 + 1) * 128], rhs=wd_sb[sl][:, ft, dc * 512:(dc + 1) * 512],
                                       start=(ei == 0 and ft == 0), stop=(ei == 3 and ft == 1), r=[f"hid{ft}", f"wd{sl}"], w=[f"bank{pob}"])
                        if tg == n_tg - 1 and e + NSLOT < 16:
                            load_expert(e + NSLOT)
                    for t2 in range(2):
                        ti = tg * 2 + t2
                        for dc in range(2):
                            pob = t2 * 2 + dc
                            op("dve", "tensor_tensor", out=xtmp[:, dc * 512:(dc + 1) * 512], in0=bk(pob), in1=modbuf[:, 5, dc * 512:(dc + 1) * 512],
                               op=ALU.mult, r=[f"bank{pob}", "mod5"], w=[f"xtmp{dc}"])
                        op("pool", "tensor_tensor", out=xacc[t2], in0=xacc[t2], in1=xtmp, op=ALU.add, r=["xtmp0", "xtmp1", f"xacc{t2}"], w=[f"xacc{t2}"])
                        rows = slice(ti * 128, (ti + 1) * 128)
                        if G < 3:
                            dma("sp", xs_d[1][rows, :], xacc[t2], r=[f"xacc{t2}"], w=[f"xs1_{ti}"])
                        elif not last_layer:
                            dma("sp", xs_d[0][rows, :], xacc[t2], r=[f"xacc{t2}"], w=[f"xs0_{ti}"])
                            if "xout" in dbg:
                                dma("sp", dbg["xout"][rows, :], xacc[t2], r=[f"xacc{t2}"], w=["dbg_xout"])
                        else:
                            if "xout" in dbg and n_layers == 1:
                                dma("sp", dbg["xout"][rows, :], xacc[t2], r=[f"xacc{t2}"], w=["dbg_xout"])
                            op("act", "activation", out=xtmp, in_=xacc[t2], func=AF.Square, accum_out=fst[:, 0:1], r=[f"xacc{t2}"], w=["xtmp0", "xtmp1", "f_ss"])
                            op("dve", "tensor_scalar", out=fst[:, 1:2], in0=fst[:, 0:1], scalar1=1.0 / D, scalar2=EPS, op0=ALU.mult, op1=ALU.add, r=["f_ss"], w=["f_ms"])
                            op("pool", "tensor_tensor", out=fst[:, 3:4], in0=fst[:, 1:2], in1=negh, op=ALU.pow, r=["f_ms", "negh"], w=["f_rstd"])
                            op("dve", "scalar_tensor_tensor", out=xtmp, in0=xacc[t2], scalar=fst[:, 3:4], in1=gfin_bc, op0=ALU.mult, op1=ALU.mult,
                               r=[f"xacc{t2}", "f_rstd", "gfin_bc"], w=["xtmp0", "xtmp1"])
                            dma("sp", y_d[rows, :], xtmp, r=["xtmp0", "xtmp1"], w=[f"y_{ti}"])
            mark('s3_done')

        if cut is not None:
            P.ops = P.ops[:marks[cut]]
        print("arena peak bytes", ar.peak, "ops", len(P.ops))
        tracks = ["pe", "act", "dve", "pool", "sp"] + [("lane", i) for i in range(N_LANES)]
        sems = {t: es.enter_context(nc.semaphore(f"sem{i}")) for i, t in enumerate(tracks)}
        P.finalize(sems)
        block = es.enter_context(nc.Block())

        @block.sync
        def _(e):
            P.emit_engine("sp", e, final_wait=True)

        @block.tensor
        def _(e):
            P.emit_engine("pe", e)

        @block.scalar
        def _(e):
            P.emit_engine("act", e)

        @block.vector
        def _(e):
            P.emit_engine("dve", e)

        @block.gpsimd
        def _(e):
            P.emit_engine("pool", e)
    return nc


def make_in_maps(inputs, n_cores=8):
    f = lambda a: np.ascontiguousarray(np.asarray(a))
    consts = _constants()
    shared = {
        "ada_w": f(inputs["ada_w"]), "ada_b": f(inputs["ada_b"]),
        "norm_mix_g": f(inputs["norm_mix_g"]), "norm_ffn_g": f(inputs["norm_ffn_g"]),
        "final_norm_g": f(inputs["final_norm_g"]).reshape(1, D),
        "w_in": f(inputs["w_in"]), "sgu_norm_g": f(inputs["sgu_norm_g"]),
        "w_sguT": f(np.asarray(inputs["w_sgu"]).transpose(0, 3, 1, 2)),
        "b_sguT": f(np.asarray(inputs["b_sgu"]).transpose(0, 2, 1)),
        "w_pool2": f(np.asarray(inputs["w_pool"]).transpose(0, 2, 1, 3)),
        "pool_scale": f(inputs["pool_scale"]),
        "w_cmp1": f(inputs["w_cmp1"]), "w_cmp2": f(inputs["w_cmp2"]),
        "cmp_peT": f(np.asarray(inputs["cmp_pe"]).transpose(0, 1, 3, 2)),
        "w_out": f(inputs["w_out"]), "router_w": f(inputs["router_w"]),
        "router_b": f(inputs["router_b"]).reshape(1, 16),
        "w_gate": f(inputs["w_gate"]), "w_up": f(inputs["w_up"]), "w_down": f(inputs["w_down"]),
    }
    shared.update(consts)
    x = np.asarray(inputs["x"])
    c = np.asarray(inputs["c"])
    pos = np.asarray(inputs["positions"]).astype(np.int32)
    maps = []
    for k in range(n_cores):
        b = k % 4
        m = dict(shared)
        m["x"] = f(x[b])
        m["c2"] = f(c[b].reshape(8, 128).T)
        m["pos2"] = f(pos[b].reshape(32, 128).T)
        pc = np.zeros(256, np.int32)
        pc[:255] = pos[b][np.arange(255) * 16 + 16]
        m["posc2"] = f(pc.reshape(2, 128).T)
        maps.append(m)
    return maps


def kernel(**inputs):
    nc = build_program()
    maps = make_in_maps(inputs)
    res = run_bass_kernel_spmd(nc, maps, core_ids=list(range(8)))
    out = np.stack([res.results[b]["y"] for b in range(4)], axis=0)
    return out.astype(np.float32)
```

```python
import numpy as np
from contextlib import ExitStack
import concourse.bass as bass
import concourse.mybir as mybir
from concourse.bass_utils import run_bass_kernel_spmd

F32 = mybir.dt.float32
BF16 = mybir.dt.bfloat16
U8 = mybir.dt.uint8
I32 = mybir.dt.int32
AF = mybir.ActivationFunctionType
ALU = mybir.AluOpType
AX = mybir.AxisListType

S = 4096
D = 1024
NT = 32
INW = 2352
EPS = 1e-6
NEGB = -32768.0
BIG = 1.0e30
SCALE = 0.125
N_LANES = 24

C_AQ, C_AK, C_AV, C_IQ, C_IK, C_IW = 0, 256, 512, 768, 896, 928
C_BU, C_BV, C_CZ = 932, 1188, 1444
C_DQ, C_KC, C_VC, C_KS, C_VS, C_KW, C_VW, C_DG = 1700, 1956, 2020, 2084, 2148, 2212, 2276, 2340


class _Op:
    __slots__ = ("eng", "fn", "reads", "writes", "lane", "idx", "deps", "signal", "seq", "is_dma")

    def __init__(self, eng, fn, reads, writes, lane):
        self.eng = eng
        self.fn = fn
        self.reads = reads
        self.writes = writes
        self.lane = lane
        self.is_dma = lane is not None
        self.deps = []
        self.signal = False
        self.seq = 0


class Prog:
    def __init__(self, nc, n_lanes):
        self.nc = nc
        self.ops = []
        self.n_lanes = n_lanes
        self._rr = 0
        self._rr_pool = 0
        self.barriers = []
        self._cap = None

    def add(self, eng, fn, reads=(), writes=(), lane=None):
        op = _Op(eng, fn, tuple(reads), tuple(writes), lane)
        (self._cap if self._cap is not None else self.ops).append(op)
        return op

    def capture(self):
        self._cap = []

    def end_capture(self):
        c, self._cap = self._cap, None
        return c

    def emit_merged(self, lists):
        keyed = []
        for li, lst in enumerate(lists):
            n = len(lst)
            for j, o in enumerate(lst):
                keyed.append(((j + 0.5) / n, li, j, o))
        keyed.sort(key=lambda t: (t[0], t[1], t[2]))
        for _, _, _, o in keyed:
            self.ops.append(o)

    def dma(self, eng, fn, reads=(), writes=()):
        half = self.n_lanes // 2
        if eng == "pool":
            lane = half + self._rr_pool
            self._rr_pool = (self._rr_pool + 1) % (self.n_lanes - half)
        else:
            lane = self._rr
            self._rr = (self._rr + 1) % half
        return self.add(eng, fn, reads, writes, lane)

    def track(self, op):
        return ("lane", op.lane) if op.is_dma else op.eng

    def barrier(self):
        self.barriers.append(len(self.ops))

    def finalize(self, sems):
        last_w, readers, lane_last = {}, {}, {}
        ops = self.ops
        last_on_track = {}
        pending = {}
        bank_last = {}
        bset = set(self.barriers)
        for n_, op in enumerate(ops):
            op.idx = n_
        for op in ops:
            deps = set()
            if op.idx in bset:
                snap = list(last_on_track.values())
                for en in ("pe", "act", "dve", "pool", "sp"):
                    pending[en] = snap
            if pending.get(op.eng):
                deps.update(pending[op.eng])
                pending[op.eng] = None
            for r in op.reads:
                if r in last_w:
                    deps.add(last_w[r])
            for w in op.writes:
                if w in last_w:
                    deps.add(last_w[w])
                for rd in readers.get(w, ()):
                    deps.add(rd)
            if op.is_dma and op.lane in lane_last:
                deps.add(lane_last[op.lane])
            bks = {k[:5] for k in op.reads + op.writes if k.startswith("bank")}
            for bkey in bks:
                d_ = bank_last.setdefault(bkey, {})
                for en, oi in d_.items():
                    if en != op.eng:
                        deps.add(oi)
                d_[op.eng] = op.idx
            deps.discard(op.idx)
            op.deps = [d for d in deps if not (op.eng == "pe" and not op.is_dma
                                               and ops[d].eng == "pe" and not ops[d].is_dma)]
            for r in op.reads:
                readers.setdefault(r, []).append(op.idx)
            for w in op.writes:
                last_w[w] = op.idx
                readers[w] = []
            if op.is_dma:
                lane_last[op.lane] = op.idx
            last_on_track[self.track(op)] = op.idx
        for op in ops:
            for d in op.deps:
                ops[d].signal = True
        cnt = {}
        for op in ops:
            t = self.track(op)
            if op.is_dma:
                op.signal = True
            if op.signal:
                cnt[t] = cnt.get(t, 0) + 1
                op.seq = cnt[t]
        self.sems = sems
        self.final_counts = cnt

    def emit_engine(self, ename, eobj, final_wait=False):
        ops, sems = self.ops, self.sems
        seen = {}
        for op in ops:
            if op.eng != ename:
                continue
            need = {}
            for d in op.deps:
                dop = ops[d]
                t = self.track(dop)
                v = dop.seq * (16 if dop.is_dma else 1)
                if need.get(t, 0) < v:
                    need[t] = v
            for t, v in need.items():
                if seen.get(t, 0) >= v:
                    continue
                eobj.wait_ge(sems[t], v)
                seen[t] = v
            ins = op.fn(eobj)
            if op.signal:
                ins.then_inc(sems[self.track(op)], 16 if op.is_dma else 1)
        if final_wait:
            for t, c in self.final_counts.items():
                v = c * (16 if isinstance(t, tuple) else 1)
                if seen.get(t, 0) < v:
                    eobj.wait_ge(sems[t], v)


class Arena:
    def __init__(self, t, nbytes):
        self.t = t
        self.nbytes = nbytes
        self.off = 0
        self.peak = 0

    def alloc(self, shape, dt):
        n = 1
        for s in shape:
            n *= s
        nb = n * mybir.dt.size(dt)
        nb_al = (nb + 63) // 64 * 64
        assert self.off + nb_al <= self.nbytes, f"arena overflow {self.off}+{nb_al}>{self.nbytes}"
        ap = self.t[:, self.off:self.off + nb].bitcast(dt)
        self.off += nb_al
        self.peak = max(self.peak, self.off)
        if len(shape) == 2:
            ap = ap.rearrange("p (a b) -> p a b", a=shape[0], b=shape[1])
        elif len(shape) == 3:
            ap = ap.rearrange("p (a b c) -> p a b c", a=shape[0], b=shape[1], c=shape[2])
        return ap

    def mark(self):
        return self.off

    def release(self, m):
        self.off = m


def _constants():
    k = {}
    t = np.arange(128)
    k["k_ident"] = np.eye(128, dtype=np.float32)
    k["k_nident"] = -np.eye(128, dtype=np.float32)
    k["k_cb"] = np.where(t[None, :] <= t[:, None], 0.0, NEGB).astype(np.float32)
    k["k_wb"] = np.where(t[None, :] > t[:, None], 0.0, NEGB).astype(np.float32)
    k["k_trilT"] = (t[None, :] >= t[:, None]).astype(np.float32)
    wins = (2, 4, 8, 16)
    mc = np.zeros((4, 128, 128), np.float32)
    mp = np.zeros((4, 128, 128), np.float32)
    m0 = np.zeros((4, 128, 128), np.float32)
    for g, w in enumerate(wins):
        for tt in range(128):
            for ss in range(max(0, tt - w + 1), tt + 1):
                mc[g, ss, tt] += 1.0 / w
            for back in range(tt + 1, w):
                mp[g, 128 - (back - tt), tt] += 1.0 / w
            cnt = min(tt + 1, w)
            for ss in range(max(0, tt - w + 1), tt + 1):
                m0[g, ss, tt] += 1.0 / cnt
            mc[g, tt, tt] -= 1.0
            m0[g, tt, tt] -= 1.0
    k["k_mcur"] = mc.transpose(1, 0, 2).copy()
    k["k_mprev"] = mp.transpose(1, 0, 2).copy()
    k["k_m0"] = m0.transpose(1, 0, 2).copy()
    k["k_d0"] = (t[None, :] - 16.0 * t[:, None]).astype(np.float32)
    n = np.arange(256)
    j = np.arange(64)
    ov = np.clip(np.minimum(j[:, None] * 64 + 64, n[None, :] * 16 + 32) - np.maximum(j[:, None] * 64, n[None, :] * 16), 0, None)
    ov = ov.astype(np.float32) / 32.0
    ov[:, 255] = 0.0
    k["k_ovT"] = ov.T.copy().reshape(2, 128, 64).transpose(1, 0, 2).copy()
    inv64 = (500000.0 ** (-np.arange(8, dtype=np.float32) / 8.0)).astype(np.float32)
    inv32 = (500000.0 ** (-np.arange(4, dtype=np.float32) / 4.0)).astype(np.float32)
    k["k_invf"] = np.tile(np.concatenate([inv64, inv32])[None, :], (128, 1)).astype(np.float32)
    sel = np.zeros((16, 16, 128), np.float32)
    for e in range(16):
        sel[e, e, :] = 1.0
    k["k_sel16"] = sel
    hm2 = np.zeros((128, 2, 128), np.float32)
    hm2[:64, 0, :] = 1.0
    hm2[64:, 1, :] = 1.0
    k["k_hm2"] = hm2
    hm4 = np.zeros((128, 4, 128), np.float32)
    for h in range(4):
        hm4[32 * h:32 * h + 32, h, :] = 1.0
    k["k_hm4"] = hm4
    k["k_pow2"] = np.tile((0.5 ** np.arange(1, 33, dtype=np.float32))[None, :], (128, 1)).astype(np.float32)
    return k


def build_program(n_tiles=NT, n_layers=2, debug=(), cut=None):
    nc = bass.Bass("TRN2", target_bir_lowering=False)

    def din(name, shape, dt=F32):
        return nc.dram_tensor(name, list(shape), dt, kind="ExternalInput").ap()

    def dscr(name, shape, dt):
        return nc.dram_tensor(name, list(shape), dt, kind="Internal").ap()

    def dout(name, shape, dt=F32):
        return nc.dram_tensor(name, list(shape), dt, kind="ExternalOutput").ap()

    x_d = din("x", [S, D])
    c_d = din("c2", [128, 8])
    pos_d = din("pos2", [128, 32], I32)
    posc_d = din("posc2", [128, 2], I32)
    adaw_d = din("ada_w", [2, D, 6 * D])
    adab_d = din("ada_b", [2, 6 * D])
    gmix_d = din("norm_mix_g", [2, D])
    gffn_d = din("norm_ffn_g", [2, D])
    gfin_d = din("final_norm_g", [1, D])
    win_d = din("w_in", [2, D, INW])
    sgug_d = din("sgu_norm_g", [2, 256])
    wsguT_d = din("w_sguT", [2, 128, 4, 128])
    bsgu_d = din("b_sguT", [2, 128, 4])
    wpool_d = din("w_pool2", [2, 64, 4, 64])
    pscale_d = din("pool_scale", [2, 256])
    wc1_d = din("w_cmp1", [2, 2, 32, 64, 64])
    wc2_d = din("w_cmp2", [2, 2, 64, 64])
    peT_d = din("cmp_peT", [2, 2, 64, 32])
    wout_d = din("w_out", [2, D, D])
    rw_d = din("router_w", [D, 16])
    rb_d = din("router_b", [1, 16])
    wg_d = din("w_gate", [2, 16, D, 256])
    wu_d = din("w_up", [2, 16, D, 256])
    wd_d = din("w_down", [2, 16, 256, D])
    kd = {}
    for name, arr in _constants().items():
        kd[name] = din(name, arr.shape)
    y_d = dout("y", [S, D])
    dbg = {}
    if "z" in debug:
        dbg["z"] = dout("dbg_z", [n_tiles * 128, INW])
    if "obc" in debug:
        dbg["obc"] = dout("dbg_obc", [n_tiles * 128, 512], BF16)
    if "cat" in debug:
        dbg["cat"] = dout("dbg_cat", [n_tiles * 128, D], BF16)
    if "xmid" in debug:
        dbg["xmid"] = dout("dbg_xmid", [n_tiles * 128, D])
    if "mba" in debug:
        dbg["mba"] = dout("dbg_mba", [128, S], BF16)
        dbg["sst"] = dout("dbg_sst", [128, 64])
        dbg["cz"] = dout("dbg_cz", [128, S], mybir.dt.float16)
        dbg["score"] = dout("dbg_score", [128, S])
    if "gate" in debug:
        dbg["gate"] = dout("dbg_gate", [n_tiles * 128, 16], BF16)
    if "xout" in debug:
        dbg["xout"] = dout("dbg_xout", [n_tiles * 128, D])
    if "mod" in debug:
        dbg["mod"] = dout("dbg_mod", [128, 6 * D])

    xs_d = dscr("xs", [2, S, D], F32)
    qscr_d = dscr("qscr", [NT, 128, 1536], BF16)
    cat_d = dscr("catscr", [NT, 128, 1024], BF16)

    es = ExitStack()
    with es:
        ARENA_BYTES = 206 * 1024
        arena_t = es.enter_context(nc.sbuf_tensor("arena", [128, ARENA_BYTES], U8))
        ar = Arena(arena_t, ARENA_BYTES)
        banks = [es.enter_context(nc.psum_tensor(f"bank{i}", [128, 512], F32)) for i in range(8)]

        def bk(i):
            return banks[i][:, :]

        def bkb(i):
            return banks[i][:, :].bitcast(BF16)

        P = Prog(nc, N_LANES)

        def op(eng, meth, r=(), w=(), **kw):
            P.add(eng, lambda e: getattr(e, meth)(**kw), r, w)

        def dma(eng, out, in_, r=(), w=()):
            P.dma(eng, lambda e: e.dma_start(out=out, in_=in_), r, w)

        marks = {}

        def mark(name):
            marks.setdefault(name, len(P.ops))

        def flat(ap):
            if len(ap.shape) == 3:
                return ap.rearrange("p a b -> p (a b)")
            return ap.rearrange("p a b c -> p (a b c)")

        ident_b = ar.alloc([128], BF16)
        ident_f = ar.alloc([128], F32)
        cb_b = ar.alloc([128], BF16)
        wb_b = ar.alloc([128], BF16)
        trilT = ar.alloc([128], F32)
        mcur = ar.alloc([4, 128], BF16)
        mprev = ar.alloc([4, 128], BF16)
        m0 = ar.alloc([4, 128], BF16)
        d0 = ar.alloc([128], F32)
        ovT = ar.alloc([2, 64], BF16)
        invf = ar.alloc([12], F32)
        pow2 = ar.alloc([32], F32)
        hm2 = ar.alloc([2, 128], BF16)
        hm4 = ar.alloc([4, 128], BF16)
        dma("pool", hm2, kd["k_hm2"][:, :, :], w=["hm2"])
        dma("pool", hm4, kd["k_hm4"][:, :, :], w=["hm4"])
        dma("pool", ident_b, kd["k_ident"][:, :], w=["ident_b"])
        nident_b = ar.alloc([128], BF16)
        negh = ar.alloc([1], F32)
        op("pool", "memset", ap=negh, constant=-0.5, w=["negh"])
        dma("pool", nident_b, kd["k_nident"][:, :], w=["nident_b"])
        dma("sp", ident_f, kd["k_ident"][:, :], w=["ident_f"])
        dma("pool", cb_b, kd["k_cb"][:, :], w=["cb_b"])
        dma("pool", wb_b, kd["k_wb"][:, :], w=["wb_b"])
        dma("sp", trilT, kd["k_trilT"][:, :], w=["trilT"])
        dma("pool", mcur, kd["k_mcur"][:, :, :], w=["mcur"])
        dma("pool", mprev, kd["k_mprev"][:, :, :], w=["mprev"])
        dma("pool", m0, kd["k_m0"][:, :, :], w=["m0"])
        dma("sp", d0, kd["k_d0"][:, :], w=["d0"])
        dma("pool", ovT, kd["k_ovT"][:, :, :], w=["ovT"])
        dma("sp", invf, kd["k_invf"][:, :], w=["invf"])
        dma("sp", pow2, kd["k_pow2"][:, :], w=["pow2"])

        sincos = ar.alloc([32, 24], F32)
        sincosc = ar.alloc([2, 16], F32)
        m_tmp = ar.mark()
        pos_i = ar.alloc([34], I32)
        pos_f = ar.alloc([34], F32)
        ang = ar.alloc([34, 12], F32)
        angs = ar.alloc([34, 24], F32)
        kq_i = ar.alloc([34, 24], I32)
        kq_f = ar.alloc([34, 24], F32)
        red = ar.alloc([34, 24], F32)
        wrp = ar.alloc([34, 24], F32)
        sc_all = ar.alloc([34, 24], F32)
        dma("sp", pos_i[:, 0:32], pos_d[:, :], w=["pos_i"])
        dma("sp", pos_i[:, 32:34], posc_d[:, :], w=["pos_ic"])
        op("dve", "tensor_copy", out=pos_f, in_=pos_i, r=["pos_i", "pos_ic"], w=["pos_f"])
        op("dve", "tensor_tensor", out=ang, in0=pos_f.unsqueeze(2).to_broadcast([128, 34, 12]),
           in1=invf.unsqueeze(1).to_broadcast([128, 34, 12]), op=ALU.mult, r=["pos_f", "invf"], w=["ang"])
        TWO_PI = float(2 * np.pi)
        op("dve", "tensor_scalar", out=angs[:, :, 0:12], in0=ang, scalar1=float(np.pi / 2), scalar2=None, op0=ALU.add,
           r=["ang"], w=["angs_c"])
        op("dve", "tensor_copy", out=angs[:, :, 12:24], in_=ang, r=["ang"], w=["angs_s"])
        op("dve", "tensor_scalar", out=kq_f, in0=angs, scalar1=float(1.0 / TWO_PI), scalar2=None, op0=ALU.mult,
           r=["angs_c", "angs_s"], w=["kq_f"])
        op("dve", "tensor_copy", out=kq_i, in_=kq_f, r=["kq_f"], w=["kq_i"])
        op("dve", "tensor_copy", out=kq_f, in_=kq_i, r=["kq_i"], w=["kq_f"])
        C1 = 6.28125
        C2 = float(TWO_PI - 6.28125)
        op("dve", "scalar_tensor_tensor", out=red, in0=kq_f, scalar=-C1, in1=angs, op0=ALU.mult, op1=ALU.add,
           r=["kq_f", "angs_c", "angs_s"], w=["red"])
        op("dve", "scalar_tensor_tensor", out=red, in0=kq_f, scalar=-C2, in1=red, op0=ALU.mult, op1=ALU.add,
           r=["kq_f", "red"], w=["red"])
        op("dve", "tensor_scalar", out=wrp, in0=red, scalar1=float(np.pi), scalar2=-TWO_PI, op0=ALU.is_ge, op1=ALU.mult,
           r=["red"], w=["wrp"])
        op("dve", "tensor_tensor", out=red, in0=red, in1=wrp, op=ALU.add, r=["red", "wrp"], w=["red"])
        op("dve", "tensor_scalar", out=wrp, in0=red, scalar1=float(-np.pi), scalar2=TWO_PI, op0=ALU.is_lt, op1=ALU.mult,
           r=["red"], w=["wrp"])
        op("dve", "tensor_tensor", out=red, in0=red, in1=wrp, op=ALU.add, r=["red", "wrp"], w=["red"])
        op("dve", "tensor_scalar", out=red, in0=red, scalar1=float(-np.pi), scalar2=float(np.pi), op0=ALU.max, op1=ALU.min,
           r=["red"], w=["red"])
        op("act", "activation", out=sc_all, in_=red, func=AF.Sin, r=["red"], w=["sc_all"])
        op("dve", "tensor_copy", out=sincos[:, :, 0:8], in_=sc_all[:, 0:32, 0:8], r=["sc_all"], w=["sincos_a"])
        op("dve", "tensor_copy", out=sincos[:, :, 8:16], in_=sc_all[:, 0:32, 12:20], r=["sc_all"], w=["sincos_b"])
        op("dve", "tensor_copy", out=sincos[:, :, 16:20], in_=sc_all[:, 0:32, 8:12], r=["sc_all"], w=["sincos_c"])
        op("dve", "tensor_copy", out=sincos[:, :, 20:24], in_=sc_all[:, 0:32, 20:24], r=["sc_all"], w=["sincos_d"])
        op("dve", "tensor_copy", out=sincosc[:, :, 0:8], in_=sc_all[:, 32:34, 0:8], r=["sc_all"], w=["sincosc_a"])
        op("dve", "tensor_copy", out=sincosc[:, :, 8:16], in_=sc_all[:, 32:34, 12:20], r=["sc_all"], w=["sincosc_b"])
        SINCOS = ["sincos_a", "sincos_b", "sincos_c", "sincos_d"]
        ar.release(m_tmp)
        P.barrier()

        mark('rope_done')
        c_sb = ar.alloc([8], F32)
        cs = ar.alloc([8], F32)
        dma("sp", c_sb, c_d[:, :], w=["c_sb"])
        op("act", "activation", out=cs, in_=c_sb, func=AF.Silu, r=["c_sb"], w=["cs"])

        modbuf = ar.alloc([6, D], F32)
        modflat = flat(modbuf)

        m_layer = ar.mark()

        for l in range(n_layers):
            ar.release(m_layer)
            P.barrier()
            kAT = ar.alloc([2, S], BF16)
            vA1 = ar.alloc([NT, 4, 65], BF16)
            ik4T = ar.alloc([S], BF16)
            ks2T = ar.alloc([S], BF16)
            kw2T = ar.alloc([S], BF16)
            kcvcT = ar.alloc([S], BF16)
            vs1 = ar.alloc([NT, 65], BF16)
            vw1 = ar.alloc([NT, 65], BF16)
            iw_all = ar.alloc([NT, 4], F32)
            gD_all = ar.alloc([NT, 12], F32)
            op("pool", "memset", ap=vA1[:, :, :, 64:65], constant=1.0, w=["vA1_ones"])
            op("pool", "memset", ap=vs1[:, :, 64:65], constant=1.0, w=["vs1_ones"])
            op("pool", "memset", ap=vw1[:, :, 64:65], constant=1.0, w=["vw1_ones"])

            if n_tiles < NT:
                op("pool", "memset", ap=kcvcT, constant=0.0, w=[f"kcvcT_{t_}" for t_ in range(NT)])
            x_src = x_d if l == 0 else xs_d[0]

            m_s0 = ar.mark()
            adaw_buf = [ar.alloc([8, 512], F32) for _ in range(2)]
            adab_buf = [ar.alloc([512], F32) for _ in range(2)]
            gbc = ar.alloc([D], F32)
            csb = ar.alloc([8, 128], F32)
            op("dve", "tensor_copy", out=csb, in_=cs.unsqueeze(2).to_broadcast([128, 8, 128]), r=["cs"], w=["csb"])
            adaw_v = adaw_d[l].rearrange("(kt p) n -> p kt n", p=128)
            for j in range(12):
                bsel = j % 2
                dma("sp", adaw_buf[bsel], adaw_v[:, :, j * 512:(j + 1) * 512], w=[f"adaw{bsel}"])
                dma("sp", adab_buf[bsel], adab_d[l:l + 1, j * 512:(j + 1) * 512].partition_broadcast(128), w=[f"adab{bsel}"])
                for kt in range(8):
                    op("pe", "matmul", out=bk(6 + bsel), lhsT=csb[:, kt, :], rhs=adaw_buf[bsel][:, kt, :], start=(kt == 0), stop=(kt == 7),
                       r=["csb", f"adaw{bsel}"], w=[f"bank{6 + bsel}"])
                op("dve", "tensor_tensor", out=modflat[:, j * 512:(j + 1) * 512], in0=bk(6 + bsel), in1=adab_buf[bsel], op=ALU.add,
                   r=[f"bank{6 + bsel}", f"adab{bsel}"], w=[f"mod{j // 2}"])
            for (slot, gd) in ((1, gmix_d), (4, gffn_d)):
                dma("sp", gbc, gd[l:l + 1, :].partition_broadcast(128), w=["gbc"])
                op("dve", "scalar_tensor_tensor", out=modbuf[:, slot, :], in0=modbuf[:, slot, :], scalar=1.0, in1=gbc,
                   op0=ALU.add, op1=ALU.mult, r=["gbc", f"mod{slot}"], w=[f"mod{slot}"])
            if "mod" in dbg and l == 0:
                dma("sp", dbg["mod"][:, :], modflat, r=[f"mod{s_}" for s_ in range(6)], w=["dbg_mod"])
            ar.release(m_s0)
            P.barrier()

            mark('s0_done')
            m_s1 = ar.mark()
            win_sb = ar.alloc([8, INW], BF16)
            win_v = win_d[l].rearrange("(kt p) n -> p kt n", p=128)
            for kt in range(8):
                dma("pool", win_sb[:, kt, :], win_v[:, kt, :], w=[f"win{kt}"])
            sgug_bc = ar.alloc([256], F32)
            pscale_bc = ar.alloc([256], F32)
            wsT = ar.alloc([4, 128], F32)
            wcT = ar.alloc([4, 128], BF16)
            bsgu = ar.alloc([4], F32)
            wpool_sb = ar.alloc([4, 64], BF16)
            dma("sp", sgug_bc, sgug_d[l:l + 1, :].partition_broadcast(128), w=["sgug_bc"])
            dma("sp", pscale_bc, pscale_d[l:l + 1, :].partition_broadcast(128), w=["pscale_bc"])
            dma("sp", wsT, wsguT_d[l], w=["wsT"])
            dma("sp", bsgu, bsgu_d[l], w=["bsgu"])
            dma("pool", wpool_sb[0:64], wpool_d[l], w=["wpool"])
            op("dve", "tensor_tensor", out=wcT, in0=wsT, in1=trilT.unsqueeze(1).to_broadcast([128, 4, 128]), op=ALU.mult,
               r=["wsT", "trilT"], w=["wcT"])

            xt = [ar.alloc([D], F32) for _ in range(2)]
            hn = ar.alloc([D], F32)
            sqj = hn
            hb = ar.alloc([D], BF16)
            hT = ar.alloc([8, 128], BF16)
            zt = ar.alloc([INW], F32)
            zb = ar.alloc([INW], BF16)
            st = ar.alloc([16], F32)
            rt = ar.alloc([4, 8, 8], F32)
            rtv = ar.alloc([4, 8, 8], F32)
            ikrep = ar.alloc([4, 32], BF16)
            ksrep = ar.alloc([2, 64], BF16)
            kwrep = ar.alloc([2, 64], BF16)
            qpack = [ar.alloc([12, 128], BF16) for _ in range(2)]
            uvg = ar.alloc([512], F32)
            vn = ar.alloc([256], F32)
            vnb = ar.alloc([256], BF16)
            zcb = [ar.alloc([256], BF16) for _ in range(2)]
            pooledT = ar.alloc([4, 128], BF16)
            obc = [ar.alloc([512], BF16) for _ in range(2)]

            print('stage1 arena off', ar.off)
            def load_x(i):
                dma("sp", xt[i % 2], x_src[i * 128:(i + 1) * 128, :], r=[f"xs0_{i}"] if l > 0 else [], w=[f"xt{i % 2}"])

            load_x(0)
            for i in range(n_tiles):
                b = i % 2
                XT = f"xt{b}"
                if i + 1 < n_tiles:
                    load_x(i + 1)
                op("act", "activation", out=sqj, in_=xt[b], func=AF.Square, accum_out=st[:, 0:1], r=[XT], w=["hn", "st0"])
                op("dve", "tensor_scalar", out=st[:, 1:2], in0=st[:, 0:1], scalar1=1.0 / D, scalar2=EPS, op0=ALU.mult, op1=ALU.add,
                   r=["st0"], w=["st1"])
                op("pool", "tensor_tensor", out=st[:, 3:4], in0=st[:, 1:2], in1=negh, op=ALU.pow, r=["st1", "negh"], w=["st3"])
                op("dve", "scalar_tensor_tensor", out=hn, in0=xt[b], scalar=st[:, 3:4], in1=modbuf[:, 1, :], op0=ALU.mult, op1=ALU.mult,
                   r=[XT, "st3", "mod1"], w=["hn"])
                op("dve", "tensor_tensor", out=hb, in0=hn, in1=modbuf[:, 0, :], op=ALU.add, r=["hn", "mod0"], w=["hb"])
                for kt in range(8):
                    op("pe", "transpose", out=bkb(0)[:, kt * 128:(kt + 1) * 128], in_=hb[:, kt * 128:(kt + 1) * 128], identity=ident_b,
                       r=["hb", "ident_b"], w=["bank0"])
                op("act", "copy", out=flat(hT), in_=bkb(0), r=["bank0"], w=["hT"])
                chunks = [(0, 512), (512, 1024), (1024, 1536), (1536, 2048), (2048, INW)]
                for ci, (n0, n1) in enumerate(chunks):
                    pb = 1 + (ci % 2)
                    for kt in range(8):
                        op("pe", "matmul", out=bk(pb)[:, 0:n1 - n0], lhsT=hT[:, kt, :], rhs=win_sb[:, kt, n0:n1], start=(kt == 0), stop=(kt == 7),
                           r=["hT", f"win{kt}"], w=[f"bank{pb}"])
                    if ci % 2 == 0:
                        op("act", "copy", out=zt[:, n0:n1], in_=bk(pb)[:, 0:n1 - n0], r=[f"bank{pb}"], w=[f"zt{ci}"])
                    else:
                        op("dve", "tensor_copy", out=zt[:, n0:n1], in_=bk(pb)[:, 0:n1 - n0], r=[f"bank{pb}"], w=[f"zt{ci}"])
                ZT = [f"zt{ci}" for ci in range(5)]
                if "z" in dbg and l == 0:
                    dma("sp", dbg["z"][i * 128:(i + 1) * 128, :], zt, r=ZT, w=["dbg_z"])

                mark('z_done')
                def rope(view, nh, half, ctab, stab, key, eng, rtb, tg_):
                    x1 = view[:, :, 0:half]
                    x2 = view[:, :, half:2 * half]
                    cb_ = ctab.unsqueeze(1).to_broadcast([128, nh, half])
                    sb_ = stab.unsqueeze(1).to_broadcast([128, nh, half])
                    t1, t2, t3, t4 = (rtb[:, q, 0:nh, 0:half] for q in range(4))
                    op(eng, "tensor_tensor", out=t1, in0=x1, in1=cb_, op=ALU.mult, r=[key] + SINCOS, w=[tg_ + "1"])
                    op(eng, "tensor_tensor", out=t2, in0=x2, in1=sb_, op=ALU.mult, r=[key] + SINCOS, w=[tg_ + "2"])
                    op(eng, "tensor_tensor", out=t3, in0=x2, in1=cb_, op=ALU.mult, r=[key] + SINCOS, w=[tg_ + "3"])
                    op(eng, "tensor_tensor", out=t4, in0=x1, in1=sb_, op=ALU.mult, r=[key] + SINCOS, w=[tg_ + "4"])
                    op(eng, "tensor_tensor", out=x1, in0=t1, in1=t2, op=ALU.subtract, r=[tg_ + "1", tg_ + "2"], w=[key])
                    op(eng, "tensor_tensor", out=x2, in0=t3, in1=t4, op=ALU.add, r=[tg_ + "3", tg_ + "4"], w=[key])

                c64, s64 = sincos[:, i, 0:8], sincos[:, i, 8:16]
                c32, s32 = sincos[:, i, 16:20], sincos[:, i, 20:24]
                rope(zt[:, C_AQ:C_AQ + 512].rearrange("p (h d) -> p h d", d=64), 8, 8, c64, s64, "zt0", "pool", rt, "rtp")
                rope(zt[:, C_IQ:C_IQ + 160].rearrange("p (h d) -> p h d", d=32), 5, 4, c32, s32, "zt1", "dve", rtv, "rtv")
                rope(zt[:, C_DQ:C_DQ + 256].rearrange("p (h d) -> p h d", d=64), 4, 8, c64, s64, "zt3", "dve", rtv, "rtv")
                rope(zt[:, C_KS:C_KS + 256].rearrange("p (h d) -> p h d", d=128), 2, 8, c64, s64, "zt4", "pool", rt, "rtp")
                mark('rope1_done')
                op("act", "copy", out=zb, in_=zt, r=ZT, w=["zb"])
                op("pool", "tensor_copy", out=vA1[:, i, :, 0:64], in_=zb[:, C_AV:C_AV + 256].rearrange("p (h d) -> p h d", d=64),
                   r=["zb"], w=[f"vA1_{i}"])
                op("pool", "tensor_copy", out=vs1[:, i, 0:64], in_=zb[:, C_VS:C_VS + 64], r=["zb"], w=[f"vs1_{i}"])
                op("pool", "tensor_copy", out=vw1[:, i, 0:64], in_=zb[:, C_VW:C_VW + 64], r=["zb"], w=[f"vw1_{i}"])
                op("pool", "tensor_copy", out=iw_all[:, i, :], in_=zt[:, C_IW:C_IW + 4], r=ZT, w=[f"iw_{i}"])
                op("act", "activation", out=gD_all[:, i, :], in_=zt[:, C_DG:C_DG + 12], func=AF.Tanh, scale=0.5, r=ZT, w=[f"gD_{i}"])
                op("pool", "tensor_scalar", out=gD_all[:, i, :], in0=gD_all[:, i, :], scalar1=0.5, scalar2=0.5, op0=ALU.mult, op1=ALU.add,
                   r=[f"gD_{i}"], w=[f"gD_{i}"])
                op("pool", "tensor_copy", out=ikrep, in_=zb[:, C_IK:C_IK + 32].unsqueeze(1).to_broadcast([128, 4, 32]), r=["zb"], w=["ikrep"])
                op("pool", "tensor_copy", out=ksrep, in_=zb[:, C_KS:C_KS + 64].unsqueeze(1).to_broadcast([128, 2, 64]), r=["zb"], w=["ksrep"])
                op("pool", "tensor_copy", out=kwrep, in_=zb[:, C_KW:C_KW + 64].unsqueeze(1).to_broadcast([128, 2, 64]), r=["zb"], w=["kwrep"])
                op("pool", "tensor_copy", out=zcb[b], in_=zb[:, C_CZ:C_CZ + 256], r=["zb"], w=[f"zcb{b}"])
                mark('stash_done')
                tsrc = [(zb[:, C_AQ:C_AQ + 128], "zb"), (zb[:, C_AQ + 128:C_AQ + 256], "zb"),
                        (zb[:, C_AK:C_AK + 128], "zb"), (zb[:, C_AK + 128:C_AK + 256], "zb"),
                        (zb[:, C_IQ:C_IQ + 128], "zb"), (flat(ikrep), "ikrep"),
                        (zb[:, C_DQ:C_DQ + 128], "zb"), (zb[:, C_DQ + 128:C_DQ + 256], "zb"),
                        (flat(ksrep), "ksrep"), (flat(kwrep), "kwrep"),
                        (zb[:, C_KC:C_KC + 128], "zb")]
                for si, (src, key) in enumerate(tsrc):
                    pbank, slot = (3, si) if si < 8 else (4, si - 8)
                    op("pe", "transpose", out=bkb(pbank)[:, slot * 128:(slot + 1) * 128], in_=src, identity=ident_b,
                       r=[key, "ident_b"], w=[f"bank{pbank}"])
                cols = slice(i * 128, (i + 1) * 128)
                qp = qpack[b]
                QP = f"qpack{b}"
                def bc2(src, n):
                    return src.unsqueeze(1).to_broadcast([128, n, 128])
                op("dve", "tensor_tensor", out=qp[:, 0:2, :], in0=bc2(bkb(3)[:, 0:128], 2), in1=hm2, op=ALU.mult, r=["bank3", "hm2"], w=[QP])
                op("dve", "tensor_tensor", out=qp[:, 2:4, :], in0=bc2(bkb(3)[:, 128:256], 2), in1=hm2, op=ALU.mult, r=["bank3", "hm2"], w=[QP])
                op("dve", "tensor_copy", out=kAT[:, :, cols], in_=bkb(3)[:, 256:512].rearrange("p (a b) -> p a b", b=128),
                   r=["bank3"], w=[f"kAT_{i}"])
                op("dve", "tensor_tensor", out=qp[:, 4:8, :], in0=bc2(bkb(3)[:, 512:640], 4), in1=hm4, op=ALU.mult, r=["bank3", "hm4"], w=[QP])
                op("dve", "tensor_copy", out=ik4T[:, cols], in_=bkb(3)[:, 640:768], r=["bank3"], w=[f"ik4T_{i}"])
                op("dve", "tensor_tensor", out=qp[:, 8:10, :], in0=bc2(bkb(3)[:, 768:896], 2), in1=hm2, op=ALU.mult, r=["bank3", "hm2"], w=[QP])
                op("dve", "tensor_tensor", out=qp[:, 10:12, :], in0=bc2(bkb(3)[:, 896:1024], 2), in1=hm2, op=ALU.mult, r=["bank3", "hm2"], w=[QP])
                op("act", "copy", out=ks2T[:, cols], in_=bkb(4)[:, 0:128], r=["bank4"], w=[f"ks2T_{i}"])
                op("act", "copy", out=kw2T[:, cols], in_=bkb(4)[:, 128:256], r=["bank4"], w=[f"kw2T_{i}"])
                op("act", "copy", out=kcvcT[:, cols], in_=bkb(4)[:, 256:384], r=["bank4"], w=[f"kcvcT_{i}"])
                dma("sp", qscr_d[i], flat(qp), r=[QP], w=[f"qscr_{i}"])

                mark('tr_done')
                op("act", "activation", out=uvg, in_=zt[:, C_BU:C_BU + 512], func=AF.Gelu_apprx_tanh, r=ZT, w=["uvg"])
                op("dve", "bn_stats", out=st[:, 4:10], in_=uvg[:, 256:512], r=["uvg"], w=["st4"])
                op("dve", "bn_aggr", out=st[:, 10:12], in_=st[:, 4:10], r=["st4"], w=["st10"])
                op("dve", "tensor_scalar", out=st[:, 12:13], in0=st[:, 11:12], scalar1=EPS, scalar2=None, op0=ALU.add, r=["st10"], w=["st12"])
                op("pool", "tensor_tensor", out=st[:, 14:15], in0=st[:, 12:13], in1=negh, op=ALU.pow, r=["st12", "negh"], w=["st14"])
                op("dve", "tensor_scalar", out=vn, in0=uvg[:, 256:512], scalar1=st[:, 10:11], scalar2=st[:, 14:15],
                   op0=ALU.subtract, op1=ALU.mult, r=["uvg", "st10", "st14"], w=["vn"])
                op("dve", "tensor_tensor", out=vnb, in0=vn, in1=sgug_bc, op=ALU.mult, r=["vn", "sgug_bc"], w=["vnb"])
                for h in range(4):
                    op("pe", "matmul", out=bk(5)[:, h * 64:(h + 1) * 64], lhsT=wcT[:, h, :], rhs=vnb[:, h * 64:(h + 1) * 64],
                       start=True, stop=True, r=["wcT", "vnb"], w=["bank5a"])
                ob = obc[b]
                OB = f"obc{b}"
                for h in range(4):
                    op("dve", "scalar_tensor_tensor", out=ob[:, h * 64:(h + 1) * 64], in0=bk(5)[:, h * 64:(h + 1) * 64],
                       scalar=bsgu[:, h:h + 1], in1=uvg[:, h * 64:(h + 1) * 64], op0=ALU.add, op1=ALU.mult,
                       r=["bank5a", "bsgu", "uvg"], w=[OB + "b"])
                mark('gmlp_done')
                for g in range(4):
                    mm = m0 if i == 0 else mcur
                    op("pe", "matmul", out=bk(6)[0:64, g * 128:(g + 1) * 128], lhsT=zcb[b][:, g * 64:(g + 1) * 64], rhs=mm[:, g, :],
                       start=True, stop=(i == 0), r=[f"zcb{b}", "m0", "mcur"], w=["bank6"])
                    if i > 0:
                        op("pe", "matmul", out=bk(6)[0:64, g * 128:(g + 1) * 128], lhsT=zcb[1 - b][:, g * 64:(g + 1) * 64], rhs=mprev[:, g, :],
                           start=False, stop=True, r=[f"zcb{1 - b}", "mprev"], w=["bank6"])
                op("act", "copy", out=flat(pooledT[0:64]), in_=bk(6)[0:64, :], r=["bank6"], w=["pooledT"])
                for g in range(4):
                    op("pe", "matmul", out=bk(5)[:, 256 + g * 64:256 + (g + 1) * 64], lhsT=pooledT[0:64, g, :], rhs=wpool_sb[0:64, g, :],
                       start=True, stop=True, r=["pooledT", "wpool"], w=["bank5b"])
                op("dve", "tensor_tensor", out=ob[:, 256:512], in0=bk(5)[:, 256:512], in1=pscale_bc, op=ALU.mult,
                   r=["bank5b", "pscale_bc"], w=[OB + "c"])
                dma("sp", cat_d[i][:, 256:768], ob, r=[OB + "b", OB + "c"], w=[f"cat_{i}"])
                if "obc" in dbg and l == 0:
                    dma("sp", dbg["obc"][i * 128:(i + 1) * 128, :], ob, r=[OB + "b", OB + "c"], w=["dbg_obc"])
            ar.release(m_s1)
            P.barrier()

            m_s2 = ar.mark()
            kc2T = ar.alloc([256], BF16)
            vcm1 = ar.alloc([2, 128], BF16)
            m_s1b = ar.mark()
            w1sb = ar.alloc([32, 64], BF16)
            w2sb = ar.alloc([2, 64], BF16)
            peTs = ar.alloc([32], BF16)
            cvec = ar.alloc([2], F32)
            hT2 = ar.alloc([2, 256], BF16)
            kcm = ar.alloc([2, 64], F32)
            kcrep = ar.alloc([2, 2, 64], BF16)
            rtc = ar.alloc([4, 2, 8], F32)
            KCVC = [f"kcvcT_{t_}" for t_ in range(n_tiles)]
            for kv in range(2):
                pr = slice(kv * 64, kv * 64 + 64)
                dma("pool", w1sb[pr], wc1_d[l][kv].rearrange("j d e -> d j e"), w=[f"w1sb{kv}"])
                dma("pool", w2sb[0:64, kv, :], wc2_d[l][kv], w=[f"w2sb{kv}"])
                dma("pool", peTs[pr], peT_d[l][kv], w=[f"peTs{kv}"])
            kv4 = kcvcT.rearrange("p (n r) -> p n r", r=16)
            op("dve", "memset", ap=kcm, constant=0.0, w=["kcm"])
            op("dve", "memset", ap=vcm1, constant=0.0, w=["vcm1"])
            for kv in range(2):
                pr = slice(kv * 64, kv * 64 + 64)
                bank = kv
                for j in range(32):
                    op("pe", "matmul", out=bk(bank)[0:64, 256:257], lhsT=w1sb[pr, j, :], rhs=peTs[pr, j:j + 1], start=(j == 0), stop=(j == 31),
                       r=[f"w1sb{kv}", f"peTs{kv}"], w=[f"bank{bank}"])
                op("dve", "tensor_copy", out=cvec[0:64, kv:kv + 1], in_=bk(bank)[0:64, 256:257], r=[f"bank{bank}"], w=[f"cvec{kv}"])
                for j in range(32):
                    rhs = kv4[pr, 0:255, j] if j < 16 else kv4[pr, 1:256, j - 16]
                    op("pe", "matmul", out=bk(bank)[0:64, 0:255], lhsT=w1sb[pr, j, :], rhs=rhs, start=(j == 0), stop=(j == 31),
                       r=[f"w1sb{kv}"] + KCVC, w=[f"bank{bank}"])
                op("act", "activation", out=hT2[0:64, kv, 0:255], in_=bk(bank)[0:64, 0:255], func=AF.Gelu_apprx_tanh,
                   bias=cvec[0:64, kv:kv + 1], scale=1.0, r=[f"bank{bank}", f"cvec{kv}"], w=[f"hT2_{kv}"])
            for c in range(2):
                size = 128 if c == 0 else 127
                n0 = c * 128
                op("pe", "matmul", out=bk(2)[0:size, c * 64:(c + 1) * 64], lhsT=hT2[0:64, 0, n0:n0 + size], rhs=w2sb[0:64, 0, :],
                   start=True, stop=True, r=["hT2_0", "w2sb0"], w=["bank2"])
                op("pe", "matmul", out=bk(2)[0:size, 128 + c * 64:128 + (c + 1) * 64], lhsT=hT2[0:64, 1, n0:n0 + size], rhs=w2sb[0:64, 1, :],
                   start=True, stop=True, r=["hT2_1", "w2sb1"], w=["bank2"])
                op("dve", "tensor_copy", out=kcm[0:size, c, :], in_=bk(2)[0:size, c * 64:(c + 1) * 64], r=["bank2"], w=["kcm"])
                op("dve", "tensor_copy", out=vcm1[0:size, c, 0:64], in_=bk(2)[0:size, 128 + c * 64:128 + (c + 1) * 64], r=["bank2"], w=["vcm1"])
            op("dve", "tensor_copy", out=vcm1[:, :, 64:128], in_=ovT, r=["ovT"], w=["vcm1"])
            kx1, kx2 = kcm[:, :, 0:8], kcm[:, :, 8:16]
            cc, ss_ = sincosc[:, :, 0:8], sincosc[:, :, 8:16]
            q1, q2, q3, q4 = (rtc[:, q_] for q_ in range(4))
            SCC = ["sincosc_a", "sincosc_b"]
            op("pool", "tensor_tensor", out=q1, in0=kx1, in1=cc, op=ALU.mult, r=["kcm"] + SCC, w=["rtc1"])
            op("pool", "tensor_tensor", out=q2, in0=kx2, in1=ss_, op=ALU.mult, r=["kcm"] + SCC, w=["rtc2"])
            op("pool", "tensor_tensor", out=q3, in0=kx2, in1=cc, op=ALU.mult, r=["kcm"] + SCC, w=["rtc3"])
            op("pool", "tensor_tensor", out=q4, in0=kx1, in1=ss_, op=ALU.mult, r=["kcm"] + SCC, w=["rtc4"])
            op("pool", "tensor_tensor", out=kx1, in0=q1, in1=q2, op=ALU.subtract, r=["rtc1", "rtc2"], w=["kcm"])
            op("pool", "tensor_tensor", out=kx2, in0=q3, in1=q4, op=ALU.add, r=["rtc3", "rtc4"], w=["kcm"])
            op("pool", "tensor_copy", out=kcrep, in_=kcm.unsqueeze(2).to_broadcast([128, 2, 2, 64]), r=["kcm"], w=["kcrep"])
            for c in range(2):
                op("pe", "transpose", out=bkb(3)[:, c * 128:(c + 1) * 128], in_=flat(kcrep[:, c]), identity=ident_b,
                   r=["kcrep", "ident_b"], w=["bank3"])
            op("dve", "tensor_copy", out=kc2T, in_=bkb(3)[:, 0:256], r=["bank3"], w=["kc2T"])
            ar.release(m_s1b)
            P.barrier()
            mark('s1b_done')

            qpk = [ar.alloc([12, 128], BF16) for _ in range(2)]
            score = ar.alloc([S], F32)
            tmpsc = [ar.alloc([512], F32) for _ in range(2)]
            z01 = ar.alloc([S], BF16)
            cz = ar.alloc([S], mybir.dt.float16)
            MBA = [ar.alloc([S], BF16) for _ in range(2)]
            MBD = [ar.alloc([S], BF16) for _ in range(2)]
            PT = [ar.alloc([4, 128], BF16) for _ in range(2)]
            PTc = ar.alloc([4, 128], BF16)
            sst = [ar.alloc([48], F32) for _ in range(2)]
            stp = ar.alloc([32], F32)
            nstp = ar.alloc([32], F32)
            bis = ar.alloc([8], F32)
            imp = ar.alloc([64], F32)
            impm = ar.alloc([64], F32)
            impw = ar.alloc([64], F32)
            m8 = ar.alloc([16], F32)
            selb = ar.alloc([64], BF16)
            bcm = ar.alloc([128], BF16)
            od1 = [ar.alloc([256], F32) for _ in range(2)]
            od2 = ar.alloc([256], F32)
            oa_st = ar.alloc([256], BF16)
            od_st = ar.alloc([256], BF16)
            coef = [ar.alloc([3, 4], F32) for _ in range(2)]
            c300 = ar.alloc([1], F32)
            op("dve", "memset", ap=c300, constant=300.0, w=["c300"])
            NIT = 22
            lcount = [0]

            def attn(i, keys, qslot0, Kf, Vf, vw_, biasf, acc_bank, accw, rk, Lbanks, PTs):
                first = True
                qp_ = qpk[i % 2]
                for n_, k_ in enumerate(keys):
                    lb = lcount[0] % 2
                    lcount[0] += 1
                    Lb = Lbanks[lb]
                    PTb, PTk = PTs[lb]
                    bias = biasf(k_)
                    for h in range(4):
                        outL = bk(Lb)[:, h * 128:(h + 1) * 128]
                        if bias is not None:
                            bap, transposed, bkeys = bias[0], bias[1], bias[2]
                            idm = nident_b if (len(bias) > 3 and bias[3]) else ident_b
                            if transposed:
                                op("pe", "matmul", out=outL, lhsT=idm, rhs=bap, start=True, stop=False, r=["ident_b", "nident_b"] + bkeys, w=[f"bank{Lb}"])
                            else:
                                op("pe", "matmul", out=outL, lhsT=bap, rhs=idm, start=True, stop=False, r=["ident_b", "nident_b"] + bkeys, w=[f"bank{Lb}"])
                        op("pe", "matmul", out=outL, lhsT=Kf(k_, h), rhs=qp_[:, qslot0 + h, :], start=(bias is None), stop=True,
                           r=rk(k_) + [f"qpk{i % 2}"], w=[f"bank{Lb}"])
                    op("act", "activation", out=flat(PTb), in_=bk(Lb), func=AF.Exp, scale=SCALE, r=[f"bank{Lb}"], w=[PTk])
                    for h in range(4):
                        op("pe", "matmul", out=bk(acc_bank)[:, h * accw:h * accw + vw_], lhsT=PTb[:, h, :], rhs=Vf(k_, h),
                           start=first, stop=(n_ == len(keys) - 1), skip_group_check=True,
                           r=[PTk] + rk(k_), w=[f"bank{acc_bank}"])
                        first = False

            def load_q(i):
                dma("sp", flat(qpk[i % 2]), qscr_d[i], r=[f"qscr_{i}"], w=[f"qpk{i % 2}"])

            def search(i):
                p = i % 2
                sp_ = sst[p]
                SS = f"sst{p}_"
                Si = 128 * (i + 1)
                qp_ = qpk[p]
                QK = f"qpk{p}"
                diag = slice(i * 128, (i + 1) * 128)
                nch = (Si + 511) // 512
                for ci in range(nch):
                    c0, c1 = ci * 512, min(Si, ci * 512 + 512)
                    wd_ = c1 - c0
                    rkeys = [f"ik4T_{t_}" for t_ in range(c0 // 128, (c1 + 127) // 128)]
                    for h in range(4):
                        pb = h % 2
                        op("pe", "matmul", out=bk(pb)[:, 0:wd_], lhsT=qp_[:, 4 + h, :], rhs=ik4T[:, c0:c1], start=True, stop=True,
                           r=[QK] + rkeys, w=[f"bank{pb}"])
                        op("act", "activation", out=tmpsc[pb][:, 0:wd_], in_=bk(pb)[:, 0:wd_], func=AF.Relu, r=[f"bank{pb}"], w=[f"tmpsc{pb}"])
                        if h == 0:
                            op("dve", "tensor_scalar", out=score[:, c0:c1], in0=tmpsc[pb][:, 0:wd_], scalar1=iw_all[:, i, 0:1], scalar2=None,
                               op0=ALU.mult, r=[f"tmpsc{pb}", f"iw_{i}"], w=[f"score{ci}"])
                        else:
                            op("dve", "scalar_tensor_tensor", out=score[:, c0:c1], in0=tmpsc[pb][:, 0:wd_], scalar=iw_all[:, i, h:h + 1],
                               in1=score[:, c0:c1], op0=ALU.mult, op1=ALU.add, r=[f"tmpsc{pb}", f"iw_{i}", f"score{ci}"], w=[f"score{ci}"])
                SC = [f"score{ci}" for ci in range(nch)]
                dci = f"score{(i * 128) // 512}"
                sc_ = score[:, 0:Si]
                if i >= 2:
                    op("dve", "tensor_reduce", out=sp_[:, 0:1], in_=sc_, axis=AX.X, op=ALU.min, r=SC, w=[SS + "lo"])
                op("dve", "tensor_tensor", out=score[:, diag], in0=score[:, diag], in1=cb_b, op=ALU.add,
                   r=[dci, "cb_b"] + ([SS + "lo"] if i >= 2 else []), w=[dci])
                if i >= 2:
                    op("dve", "tensor_reduce", out=sp_[:, 1:2], in_=sc_, axis=AX.X, op=ALU.max, r=SC, w=[SS + "hi"])
                    op("dve", "tensor_tensor", out=sp_[:, 2:3], in0=sp_[:, 1:2], in1=sp_[:, 0:1], op=ALU.subtract, r=[SS + "lo", SS + "hi"], w=[SS + "rng"])
                    op("dve", "tensor_scalar", out=stp[:, 0:NIT], in0=pow2[:, 0:NIT], scalar1=sp_[:, 2:3], scalar2=None, op0=ALU.mult,
                       r=[SS + "rng", "pow2"], w=["stp"])
                    op("dve", "tensor_scalar", out=nstp[:, 0:NIT], in0=stp[:, 0:NIT], scalar1=-1.0, scalar2=None, op0=ALU.mult, r=["stp"], w=["nstp"])
                    op("dve", "scalar_tensor_tensor", out=bis[:, 0:1], in0=sp_[:, 0:1], scalar=-1.0, in1=nstp[:, 0:1], op0=ALU.mult, op1=ALU.add,
                       r=[SS + "lo", "nstp"], w=["b_nT"])
                ncc = 2 if i >= 16 else 1
                cmp_bias = {}
                for c in range(ncc):
                    thr_ic = 2048.0 * c + 31.0 - 128.0 * i
                    if thr_ic > -2032.0:
                        cmp_bias[c] = thr_ic

                def cmp_biasf(c):
                    if c not in cmp_bias:
                        return None
                    op("pool", "tensor_scalar", out=bcm, in0=d0, scalar1=float(cmp_bias[c]), scalar2=NEGB, op0=ALU.is_lt, op1=ALU.mult,
                       r=["d0"], w=["bcm"])
                    return (bcm, True, ["bcm"])
                attn(i, list(range(ncc)), 8, lambda c, h: kc2T[:, c * 128:(c + 1) * 128], lambda c, h: vcm1[:, c, :], 128,
                     cmp_biasf, 5, 128, lambda c: ["kc2T", "vcm1"], [0, 1], [(PTc, "PTc"), (PTc, "PTc")])
                if i >= 2:
                    a_ = max(64, int(Si * 0.55) // 64 * 64)
                    nA = Si - a_
                    op("act", "activation", out=z01[:, 0:Si], in_=sc_, func=AF.Sign, bias=-1.0e-30, scale=1.0, accum_out=sp_[:, 40:41],
                       r=SC, w=["z01", SS + "sump"])
                    op("act", "activation", out=z01[:, 0:Si], in_=sc_, func=AF.Sign, bias=1.0e-30, scale=1.0, accum_out=sp_[:, 41:42],
                       r=SC, w=["z01", SS + "sumn"])
                    op("dve", "tensor_tensor", out=sp_[:, 3:4], in0=sp_[:, 0:1], in1=stp[:, 0:1], op=ALU.add, r=[SS + "lo", "stp"], w=[SS + "mid"])
                    for k_ in range(NIT):
                        op("act", "activation", out=z01[:, a_:Si], in_=score[:, a_:Si], func=AF.Sign, bias=sp_[:, 3:4], scale=-1.0, accum_out=sp_[:, 42:43],
                           r=SC + [SS + "mid"], w=["z01", SS + "sga"])
                        op("dve", "tensor_scalar", out=cz[:, 0:a_], in0=score[:, 0:a_], scalar1=sp_[:, 3:4], scalar2=0.0, op0=ALU.is_ge, op1=ALU.add,
                           accum_out=sp_[:, 4:5], r=SC + [SS + "mid"], w=["cz", SS + "cnt"])
                        op("act", "activation", out=sp_[:, 43:44], in_=sp_[:, 42:43], func=AF.Identity, scale=0.5, bias=float(255.5 - nA / 2.0),
                           r=[SS + "sga"], w=[SS + "tot"])
                        op("dve", "tensor_scalar", out=sp_[:, 5:6], in0=sp_[:, 4:5], scalar1=sp_[:, 43:44], scalar2=stp[:, k_:k_ + 1],
                           op0=ALU.is_ge, op1=ALU.mult, r=[SS + "cnt", SS + "tot", "stp"], w=[SS + "delta"])
                        if k_ < NIT - 1:
                            op("dve", "scalar_tensor_tensor", out=sp_[:, 3:4], in0=sp_[:, 3:4], scalar=stp[:, k_ + 1:k_ + 2], in1=sp_[:, 5:6],
                               op0=ALU.subtract, op1=ALU.add, r=[SS + "mid", "stp", SS + "delta"], w=[SS + "mid"])
                        else:
                            op("dve", "scalar_tensor_tensor", out=sp_[:, 6:7], in0=sp_[:, 3:4], scalar=stp[:, k_:k_ + 1], in1=sp_[:, 5:6],
                               op0=ALU.subtract, op1=ALU.add, r=[SS + "mid", "stp", SS + "delta"], w=[SS + "thr"])
                MA = f"MBA{p}"
                if i >= 2:
                    op("dve", "tensor_scalar", out=sp_[:, 32:33], in0=sp_[:, 40:41], scalar1=float(Si), scalar2=0.5, op0=ALU.add, op1=ALU.mult,
                       r=[SS + "sump"], w=[SS + "cpos"])
                    op("dve", "tensor_scalar", out=sp_[:, 33:34], in0=sp_[:, 41:42], scalar1=float(Si), scalar2=0.5, op0=ALU.add, op1=ALU.mult,
                       r=[SS + "sumn"], w=[SS + "cnn"])
                    op("dve", "tensor_scalar", out=sp_[:, 34:35], in0=sp_[:, 32:33], scalar1=255.5, scalar2=None, op0=ALU.is_lt, r=[SS + "cpos"], w=[SS + "tfa"])
                    op("dve", "scalar_tensor_tensor", out=sp_[:, 35:36], in0=sp_[:, 33:34], scalar=255.5, in1=sp_[:, 34:35], op0=ALU.is_gt, op1=ALU.mult,
                       r=[SS + "cnn", SS + "tfa"], w=[SS + "tf"])
                    op("dve", "tensor_scalar", out=sp_[:, 36:37], in0=sp_[:, 32:33], scalar1=-1.0, scalar2=256.0, op0=ALU.mult, op1=ALU.add,
                       r=[SS + "cpos"], w=[SS + "need0"])
                    op("dve", "tensor_tensor", out=sp_[:, 37:38], in0=sp_[:, 36:37], in1=sp_[:, 35:36], op=ALU.mult, r=[SS + "need0", SS + "tf"], w=[SS + "need"])
                    op("dve", "tensor_scalar", out=sp_[:, 38:39], in0=sp_[:, 35:36], scalar1=-1.0, scalar2=1.0, op0=ALU.mult, op1=ALU.add,
                       r=[SS + "tf"], w=[SS + "ntf"])
                    op("dve", "tensor_tensor", out=sp_[:, 6:7], in0=sp_[:, 6:7], in1=sp_[:, 38:39], op=ALU.mult, r=[SS + "thr", SS + "ntf"], w=[SS + "thr"])
                    op("dve", "scalar_tensor_tensor", out=sp_[:, 6:7], in0=sp_[:, 35:36], scalar=1.0e-30, in1=sp_[:, 6:7], op0=ALU.mult, op1=ALU.add,
                       r=[SS + "tf", SS + "thr"], w=[SS + "thr"])
                    op("dve", "tensor_scalar", out=z01[:, 0:Si], in0=sc_, scalar1=0.0, scalar2=None, op0=ALU.is_equal, r=SC, w=["z01"])
                    op("dve", "tensor_tensor_scan", out=cz[:, 0:Si], data0=z01[:, 0:Si], data1=c300[:, 0:1].to_broadcast([128, Si]), initial=0.0,
                       op0=ALU.add, op1=ALU.min, r=["z01", "c300"], w=["cz"])
                    op("dve", "scalar_tensor_tensor", out=z01[:, 0:Si], in0=cz[:, 0:Si], scalar=sp_[:, 37:38], in1=z01[:, 0:Si], op0=ALU.is_le, op1=ALU.mult,
                       r=["cz", SS + "need", "z01"], w=["z01"])
                    op("dve", "scalar_tensor_tensor", out=sc_, in0=z01[:, 0:Si], scalar=1.0e-20, in1=sc_, op0=ALU.mult, op1=ALU.add,
                       r=["z01"] + SC, w=SC)
                else:
                    op("dve", "memset", ap=sp_[:, 6:7], constant=-1.0e4, w=[SS + "thr"])
                op("dve", "tensor_scalar", out=sp_[:, 44:45], in0=sp_[:, 6:7], scalar1=1.0e33, scalar2=None, op0=ALU.mult, r=[SS + "thr"], w=[SS + "thrL"])
                op("act", "activation", out=MBA[p][:, 0:Si], in_=sc_, func=AF.Relu, bias=sp_[:, 44:45], scale=-1.0e33, r=SC + [SS + "thrL"], w=[MA])
                pcm = bk(5).rearrange("p (h w) -> p h w", w=128)
                op("dve", "tensor_reduce", out=sp_[:, 8:12], in_=pcm[:, :, 64:128], axis=AX.X, op=ALU.add, r=["bank5"], w=[SS + "zc"])
                op("dve", "tensor_scalar", out=sp_[:, 8:12], in0=sp_[:, 8:12], scalar1=1.0e-30, scalar2=None, op0=ALU.max, r=[SS + "zc"], w=[SS + "zc"])
                op("dve", "reciprocal", out=sp_[:, 12:16], in_=sp_[:, 8:12], r=[SS + "zc"], w=[SS + "rzc"])
                op("dve", "tensor_scalar", out=imp, in0=pcm[:, 0, 64:128], scalar1=sp_[:, 12:13], scalar2=None, op0=ALU.mult, r=["bank5", SS + "rzc"], w=["imp"])
                for h in range(1, 4):
                    op("dve", "scalar_tensor_tensor", out=imp, in0=pcm[:, h, 64:128], scalar=sp_[:, 12 + h:13 + h], in1=imp, op0=ALU.mult, op1=ALU.add,
                       r=["bank5", SS + "rzc", "imp"], w=["imp"])
                gv = gD_all[:, i, :].rearrange("p (h g) -> p h g", g=3)

                def bc4(ap):
                    return ap.unsqueeze(2).to_broadcast([128, 4, 64])
                op("dve", "tensor_tensor", out=coef[p][:, 0, :], in0=gv[:, :, 0], in1=sp_[:, 12:16], op=ALU.mult, r=[f"gD_{i}", SS + "rzc"], w=[f"coef0_{p}"])
                op("dve", "tensor_tensor", out=od1[p].rearrange("p (h d) -> p h d", d=64), in0=pcm[:, :, 0:64], in1=bc4(coef[p][:, 0, :]), op=ALU.mult,
                   r=["bank5", f"coef0_{p}"], w=[f"od1_{p}"])
                op("dve", "tensor_copy", out=impm, in_=imp, r=["imp"], w=["impm"])
                op("dve", "memset", ap=impm[:, 0:1], constant=BIG, r=["impm"], w=["impm"])
                if 2 * i + 2 < 64:
                    op("dve", "memset", ap=impm[:, 2 * i + 2:64], constant=-BIG, r=["impm"], w=["impm"])
                op("dve", "memset", ap=impm[0:64, 2 * i:2 * i + 1], constant=BIG, r=["impm"], w=["impm"])
                op("dve", "memset", ap=impm[0:64, 2 * i + 1:2 * i + 2], constant=-BIG, r=["impm"], w=["impm"])
                op("dve", "memset", ap=impm[64:128, 2 * i + 1:2 * i + 2], constant=BIG, r=["impm"], w=["impm"])
                op("dve", "max", out=m8[:, 0:8], in_=impm, r=["impm"], w=["m8a"])
                op("dve", "match_replace", out=impw, in_to_replace=m8[:, 0:8], in_values=impm, imm_value=-BIG, r=["impm", "m8a"], w=["impw"])
                op("dve", "max", out=m8[:, 8:16], in_=impw, r=["impw"], w=["m8b"])
                op("dve", "tensor_scalar", out=sp_[:, 16:17], in0=m8[:, 15:16], scalar1=-1.0e29, scalar2=None, op0=ALU.max, r=["m8b"], w=[SS + "t16"])
                op("dve", "tensor_scalar", out=selb, in0=impm, scalar1=sp_[:, 16:17], scalar2=NEGB, op0=ALU.is_lt, op1=ALU.mult,
                   r=["impm", SS + "t16"], w=["selb"])
                nb_ = 2 * (i + 1)
                MD = f"MBD{p}"
                op("dve", "tensor_copy", out=MBD[p][:, 0:Si].rearrange("p (j r) -> p j r", r=64),
                   in_=selb[:, 0:nb_].unsqueeze(2).to_broadcast([128, nb_, 64]), r=["selb"], w=[MD])
                op("dve", "tensor_tensor", out=MBD[p][:, diag], in0=MBD[p][:, diag], in1=cb_b, op=ALU.add, r=[MD, "cb_b"], w=[MD])
                if "mba" in dbg and l == 0 and i == n_tiles - 1:
                    dma("sp", dbg["mba"][:, :], MBA[p], r=[MA], w=["dbg_mba"])

            def attend(i):
                p = i % 2
                sp_ = sst[p]
                SS = f"sst{p}_"
                KT = list(range(i + 1))
                LB = [2, 3]
                PTS = [(PT[0], "PT0"), (PT[1], "PT1")]
                attn(i, KT, 0, lambda k_, h: kAT[:, h // 2, k_ * 128:(k_ + 1) * 128], lambda k_, h: vA1[:, k_, h, :], 65,
                     lambda k_: (MBA[p][:, k_ * 128:(k_ + 1) * 128], False, [f"MBA{p}"], True), 4, 65,
                     lambda k_: [f"kAT_{k_}", f"vA1_{k_}", "vA1_ones"], LB, PTS)
                WK = list(range(max(0, i - 4), i + 1))

                def win_biasf(k_):
                    if k_ == i:
                        return (cb_b, False, ["cb_b"])
                    if k_ == i - 4:
                        return (wb_b, False, ["wb_b"])
                    return None
                attn(i, WK, 8, lambda k_, h: kw2T[:, k_ * 128:(k_ + 1) * 128], lambda k_, h: vw1[:, k_, :], 65,
                     win_biasf, 7, 65, lambda k_: [f"kw2T_{k_}", f"vw1_{k_}", "vw1_ones"], LB, PTS)
                attn(i, KT, 8, lambda k_, h: ks2T[:, k_ * 128:(k_ + 1) * 128], lambda k_, h: vs1[:, k_, :], 65,
                     lambda k_: (MBD[p][:, k_ * 128:(k_ + 1) * 128], False, [f"MBD{p}"]), 6, 65,
                     lambda k_: [f"ks2T_{k_}", f"vs1_{k_}", "vs1_ones"], LB, PTS)
                pA = bk(4)[:, 0:260].rearrange("p (h w) -> p h w", w=65)
                pS = bk(6)[:, 0:260].rearrange("p (h w) -> p h w", w=65)
                pW = bk(7)[:, 0:260].rearrange("p (h w) -> p h w", w=65)

                def bc4(ap):
                    return ap.unsqueeze(2).to_broadcast([128, 4, 64])
                op("dve", "reciprocal", out=sp_[:, 20:24], in_=pA[:, :, 64], r=["bank4"], w=[SS + "rza"])
                op("dve", "tensor_tensor", out=oa_st.rearrange("p (h d) -> p h d", d=64), in0=pA[:, :, 0:64], in1=bc4(sp_[:, 20:24]),
                   op=ALU.mult, r=["bank4", SS + "rza"], w=["oa_st"])
                dma("sp", cat_d[i][:, 0:256], oa_st, r=["oa_st"], w=[f"cata_{i}"])
                op("dve", "reciprocal", out=sp_[:, 24:28], in_=pS[:, :, 64], r=["bank6"], w=[SS + "rzs"])
                op("dve", "reciprocal", out=sp_[:, 28:32], in_=pW[:, :, 64], r=["bank7"], w=[SS + "rzw"])
                gv = gD_all[:, i, :].rearrange("p (h g) -> p h g", g=3)
                op("dve", "tensor_tensor", out=coef[p][:, 1, :], in0=gv[:, :, 1], in1=sp_[:, 24:28], op=ALU.mult, r=[f"gD_{i}", SS + "rzs"], w=[f"coef1_{p}"])
                op("dve", "tensor_tensor", out=coef[p][:, 2, :], in0=gv[:, :, 2], in1=sp_[:, 28:32], op=ALU.mult, r=[f"gD_{i}", SS + "rzw"], w=[f"coef2_{p}"])
                o2 = od2.rearrange("p (h d) -> p h d", d=64)
                op("dve", "tensor_tensor", out=o2, in0=pS[:, :, 0:64], in1=bc4(coef[p][:, 1, :]), op=ALU.mult, r=["bank6", f"coef1_{p}"], w=["od2"])
                op("pool", "tensor_tensor", out=od1[p], in0=od1[p], in1=od2, op=ALU.add, r=[f"od1_{p}", "od2"], w=[f"od1_{p}"])
                op("dve", "tensor_tensor", out=o2, in0=pW[:, :, 0:64], in1=bc4(coef[p][:, 2, :]), op=ALU.mult, r=["bank7", f"coef2_{p}"], w=["od2"])
                op("pool", "tensor_tensor", out=od_st, in0=od1[p], in1=od2, op=ALU.add, r=[f"od1_{p}", "od2"], w=["od_st"])
                dma("sp", cat_d[i][:, 768:1024], od_st, r=["od_st"], w=[f"catd_{i}"])

            print('stage2 arena off', ar.off)
            load_q(0)
            search(0)
            for i in range(n_tiles):
                if i + 1 < n_tiles:
                    load_q(i + 1)
                    P.capture()
                    search(i + 1)
                    ys = P.end_capture()
                    P.capture()
                    attend(i)
                    xs_ = P.end_capture()
                    P.emit_merged([xs_, ys])
                else:
                    attend(i)
            mark('s2_done')
            ar.release(m_layer)
            P.barrier()

            h2T = ar.alloc([8, S], BF16)
            gateT = ar.alloc([S], BF16)
            sel16 = ar.alloc([16, 128], BF16)
            gfin_bc = ar.alloc([D], F32)
            dma("pool", sel16[0:16], kd["k_sel16"][:, :, :], w=["sel16"])
            dma("sp", gfin_bc, gfin_d[0:1, :].partition_broadcast(128), w=["gfin_bc"])
            m_s3 = ar.mark()
            rw32 = ar.alloc([8, 16], F32)
            rb_bc = ar.alloc([16], F32)
            dma("sp", rw32, rw_d.rearrange("(kt p) e -> p kt e", p=128), w=["rw32"])
            dma("sp", rb_bc, rb_d[0:1, :].partition_broadcast(128), w=["rb_bc"])

            wout_sb = ar.alloc([8, D], BF16)
            wout_v = wout_d[l].rearrange("(kt p) n -> p kt n", p=128)
            for kt in range(8):
                dma("pool", wout_sb[:, kt, :], wout_v[:, kt, :], w=[f"wout{kt}"])
            catl = [ar.alloc([D], BF16) for _ in range(4)]
            catT = [ar.alloc([8, 128], BF16) for _ in range(2)]
            xin = [ar.alloc([D], F32) for _ in range(4)]
            xa = [ar.alloc([D], F32) for _ in range(4)]
            h2 = [ar.alloc([D], F32) for _ in range(2)]
            h2b = [ar.alloc([D], BF16) for _ in range(2)]
            h2T32 = [ar.alloc([8, 128], F32) for _ in range(2)]
            rs2 = [ar.alloc([128], F32) for _ in range(2)]
            gateb2 = [ar.alloc([16], BF16) for _ in range(2)]
            print('stage3a arena off', ar.off)

            def load_xa(i):
                q = i % 4
                dma("sp", catl[q], cat_d[i], r=[f"cat_{i}", f"cata_{i}", f"catd_{i}"], w=[f"catl{q}"])
                dma("sp", xin[q], x_src[i * 128:(i + 1) * 128, :], r=[f"xs0_{i}"] if l > 0 else [], w=[f"xin{q}"])

            def body3a(i):
                b = i % 2
                q = i % 4
                B0 = 4 * b
                XA = f"xa{q}"
                sfx = f"_{b}"
                rs_ = rs2[b]
                gateb = gateb2[b]
                cols = slice(i * 128, (i + 1) * 128)
                for kt in range(8):
                    op("pe", "transpose", out=bkb(B0)[:, kt * 128:(kt + 1) * 128], in_=catl[q][:, kt * 128:(kt + 1) * 128], identity=ident_b,
                       r=[f"catl{q}", "ident_b"], w=[f"bank{B0}"])
                op("act", "copy", out=flat(catT[b]), in_=bkb(B0), r=[f"bank{B0}"], w=["catT" + sfx])
                for nc_ in range(2):
                    pb = B0 + 1 + nc_
                    for kt in range(8):
                        op("pe", "matmul", out=bk(pb), lhsT=catT[b][:, kt, :], rhs=wout_sb[:, kt, nc_ * 512:(nc_ + 1) * 512],
                           start=(kt == 0), stop=(kt == 7), r=["catT" + sfx, f"wout{kt}"], w=[f"bank{pb}"])
                    op("dve", "tensor_tensor", out=xa[q][:, nc_ * 512:(nc_ + 1) * 512], in0=bk(pb), in1=modbuf[:, 2, nc_ * 512:(nc_ + 1) * 512],
                       op=ALU.mult, r=[f"bank{pb}", "mod2"], w=[XA])
                op("pool", "tensor_tensor", out=xa[q], in0=xa[q], in1=xin[q], op=ALU.add, r=[XA, f"xin{q}"], w=[XA])
                dma("sp", xs_d[1][i * 128:(i + 1) * 128, :], xa[q], r=[XA], w=[f"xs1_{i}"])
                if "xmid" in dbg and l == 0:
                    dma("sp", dbg["xmid"][i * 128:(i + 1) * 128, :], xa[q], r=[XA], w=["dbg_xmid"])
                H2 = "h2" + sfx
                op("act", "activation", out=h2[b], in_=xa[q], func=AF.Square, accum_out=rs_[:, 0:1], r=[XA], w=[H2, "r_ss" + sfx])
                op("dve", "tensor_scalar", out=rs_[:, 1:2], in0=rs_[:, 0:1], scalar1=1.0 / D, scalar2=EPS, op0=ALU.mult, op1=ALU.add, r=["r_ss" + sfx], w=["r_ms" + sfx])
                op("pool", "tensor_tensor", out=rs_[:, 3:4], in0=rs_[:, 1:2], in1=negh, op=ALU.pow, r=["r_ms" + sfx, "negh"], w=["r_rstd" + sfx])
                op("dve", "scalar_tensor_tensor", out=h2[b], in0=xa[q], scalar=rs_[:, 3:4], in1=modbuf[:, 4, :], op0=ALU.mult, op1=ALU.mult,
                   r=[XA, "r_rstd" + sfx, "mod4"], w=[H2])
                op("dve", "tensor_tensor", out=h2[b], in0=h2[b], in1=modbuf[:, 3, :], op=ALU.add, r=[H2, "mod3"], w=[H2])
                op("act", "copy", out=h2b[b], in_=h2[b], r=[H2], w=["h2b" + sfx])
                for kt in range(8):
                    op("pe", "transpose", out=bkb(B0)[:, kt * 128:(kt + 1) * 128], in_=h2b[b][:, kt * 128:(kt + 1) * 128], identity=ident_b,
                       r=["h2b" + sfx, "ident_b"], w=[f"bank{B0}"])
                op("act", "copy", out=h2T[:, :, cols], in_=bkb(B0).rearrange("p (a b) -> p a b", b=128), r=[f"bank{B0}"], w=[f"h2T_{i}"])
                for kt in range(8):
                    pb = B0 + 1 + kt // 4
                    op("pe", "transpose", out=bk(pb)[:, (kt % 4) * 128:(kt % 4 + 1) * 128], in_=h2[b][:, kt * 128:(kt + 1) * 128], identity=ident_f,
                       r=[H2, "ident_f"], w=[f"bank{pb}"])
                op("dve", "tensor_copy", out=flat(h2T32[b][:, 0:4, :]), in_=bk(B0 + 1), r=[f"bank{B0 + 1}"], w=["h2T32a" + sfx])
                op("dve", "tensor_copy", out=flat(h2T32[b][:, 4:8, :]), in_=bk(B0 + 2), r=[f"bank{B0 + 2}"], w=["h2T32b" + sfx])
                RB = B0 + 3
                for kt in range(8):
                    op("pe", "matmul", out=bk(RB)[:, 0:16], lhsT=h2T32[b][:, kt, :], rhs=rw32[:, kt, :], start=(kt == 0), stop=(kt == 7),
                       r=["h2T32a" + sfx, "h2T32b" + sfx, "rw32"], w=[f"bank{RB}"])
                aff = rs_[:, 16:32]
                bia = rs_[:, 32:48]
                bv = bia.rearrange("p (g e) -> p g e", e=4)
                eq = rs_[:, 48:64]
                eqv = eq.rearrange("p (g e) -> p g e", e=4)
                mb = rs_[:, 64:80]
                mbv = mb.rearrange("p (g e) -> p g e", e=4)
                K_ = lambda nm: nm + sfx
                op("act", "activation", out=aff, in_=bk(RB)[:, 0:16], func=AF.Sigmoid, r=[f"bank{RB}"], w=[K_("r_aff")])
                op("dve", "tensor_tensor", out=bia, in0=aff, in1=rb_bc, op=ALU.add, r=[K_("r_aff"), "rb_bc"], w=[K_("r_bia")])
                op("dve", "tensor_reduce", out=rs_[:, 4:8], in_=bv, axis=AX.X, op=ALU.max, r=[K_("r_bia")], w=[K_("r_m1")])
                op("dve", "tensor_tensor", out=eqv, in0=bv, in1=rs_[:, 4:8].unsqueeze(2).to_broadcast([128, 4, 4]), op=ALU.is_equal,
                   r=[K_("r_bia"), K_("r_m1")], w=[K_("r_eq")])
                op("dve", "scalar_tensor_tensor", out=eq, in0=eq, scalar=-BIG, in1=bia, op0=ALU.mult, op1=ALU.add, r=[K_("r_eq"), K_("r_bia")], w=[K_("r_eq")])
                op("dve", "tensor_reduce", out=rs_[:, 8:12], in_=eqv, axis=AX.X, op=ALU.max, r=[K_("r_eq")], w=[K_("r_m2")])
                op("dve", "tensor_tensor", out=rs_[:, 8:12], in0=rs_[:, 8:12], in1=rs_[:, 4:8], op=ALU.add, r=[K_("r_m1"), K_("r_m2")], w=[K_("r_gs")])
                op("dve", "tensor_reduce", out=rs_[:, 12:13], in_=rs_[:, 8:12], axis=AX.X, op=ALU.max, r=[K_("r_gs")], w=[K_("r_gmax")])
                op("dve", "tensor_scalar", out=rs_[:, 8:12], in0=rs_[:, 8:12], scalar1=rs_[:, 12:13], scalar2=-BIG, op0=ALU.is_lt, op1=ALU.mult,
                   r=[K_("r_gs"), K_("r_gmax")], w=[K_("r_pen")])
                op("dve", "tensor_tensor", out=mbv, in0=bv, in1=rs_[:, 8:12].unsqueeze(2).to_broadcast([128, 4, 4]), op=ALU.add,
                   r=[K_("r_bia"), K_("r_pen")], w=[K_("r_mb")])
                op("dve", "max", out=rs_[:, 80:88], in_=mb, r=[K_("r_mb")], w=[K_("r_m8")])
                op("dve", "tensor_scalar", out=eq, in0=mb, scalar1=rs_[:, 81:82], scalar2=None, op0=ALU.is_ge, r=[K_("r_mb"), K_("r_m8"), K_("r_eq")], w=[K_("r_sel")])
                op("dve", "tensor_tensor", out=eq, in0=eq, in1=aff, op=ALU.mult, r=[K_("r_sel"), K_("r_aff")], w=[K_("r_ta")])
                op("dve", "tensor_reduce", out=rs_[:, 13:14], in_=eq, axis=AX.X, op=ALU.add, r=[K_("r_ta")], w=[K_("r_ws")])
                op("dve", "reciprocal", out=rs_[:, 14:15], in_=rs_[:, 13:14], r=[K_("r_ws")], w=[K_("r_rws")])
                op("dve", "tensor_scalar", out=gateb, in0=eq, scalar1=rs_[:, 14:15], scalar2=None, op0=ALU.mult, r=[K_("r_ta"), K_("r_rws")], w=[K_("gateb")])
                op("pe", "transpose", out=bkb(RB)[0:16, 512:640], in_=gateb, identity=ident_b, r=[K_("gateb"), "ident_b"], w=[f"bank{RB}"])
                op("dve", "tensor_copy", out=gateT[0:16, cols], in_=bkb(RB)[0:16, 512:640], r=[f"bank{RB}"], w=[f"gateT_{i}"])
                if "gate" in dbg and l == 0:
                    dma("sp", dbg["gate"][i * 128:(i + 1) * 128, :], gateb, r=[K_("gateb")], w=["dbg_gate"])

            load_xa(0)
            if n_tiles > 1:
                load_xa(1)
            for i in range(0, n_tiles, 2):
                for t_ in (i + 2, i + 3):
                    if t_ < n_tiles:
                        load_xa(t_)
                P.capture()
                body3a(i)
                s_a = P.end_capture()
                if i + 1 < n_tiles:
                    P.capture()
                    body3a(i + 1)
                    s_b = P.end_capture()
                    P.emit_merged([s_a, s_b])
                else:
                    P.emit_merged([s_a])
            ar.release(m_s3)
            P.barrier()
            mark('s3a_done')

            NSLOT = 6
            wg_sb = [ar.alloc([8, 256], BF16) for _ in range(NSLOT)]
            wu_sb = [ar.alloc([8, 256], BF16) for _ in range(NSLOT)]
            wd_sb = [ar.alloc([2, D], BF16) for _ in range(NSLOT)]
            sg = [ar.alloc([256], F32) for _ in range(2)]
            hu = [ar.alloc([256], F32) for _ in range(2)]
            hid = [ar.alloc([256], BF16) for _ in range(2)]
            xacc = [ar.alloc([D], F32) for _ in range(2)]
            xtmp = ar.alloc([D], F32)
            fst = ar.alloc([8], F32)
            print('stage3b arena off', ar.off)
            TG = 256
            n_tg = (n_tiles * 128) // TG

            def load_expert(e):
                sl = e % NSLOT
                dma("pool", wg_sb[sl], wg_d[l][e].rearrange("(kt p) f -> p kt f", p=128), w=[f"wg{sl}"])
                dma("pool", wu_sb[sl], wu_d[l][e].rearrange("(kt p) f -> p kt f", p=128), w=[f"wu{sl}"])
                dma("pool", wd_sb[sl], wd_d[l][e].rearrange("(ft p) d -> p ft d", p=128), w=[f"wd{sl}"])

            for e in range(min(NSLOT, 16)):
                load_expert(e)
            last_layer = (l == n_layers - 1)
            for G in range(4):
                for tg in range(n_tg):
                    tcols = slice(tg * TG, (tg + 1) * TG)
                    HK = [f"h2T_{tg * 2}", f"h2T_{tg * 2 + 1}"]
                    GK = [f"gateT_{tg * 2}", f"gateT_{tg * 2 + 1}"]
                    for t2 in range(2):
                        ti = tg * 2 + t2
                        dma("sp", xacc[t2], xs_d[1][ti * 128:(ti + 1) * 128, :], r=[f"xs1_{ti}"], w=[f"xacc{t2}"])
                    for ei in range(4):
                        e = G * 4 + ei
                        sl = e % NSLOT
                        for ft in range(2):
                            pb = 4 + ft
                            for kt in range(8):
                                op("pe", "matmul", out=bk(pb)[:, 0:256], lhsT=wg_sb[sl][:, kt, ft * 128:(ft + 1) * 128], rhs=h2T[:, kt, tcols],
                                   start=(kt == 0), stop=(kt == 7), r=[f"wg{sl}"] + HK, w=[f"bank{pb}"])
                            for kt in range(8):
                                op("pe", "matmul", out=bk(pb)[:, 256:512], lhsT=wu_sb[sl][:, kt, ft * 128:(ft + 1) * 128], rhs=h2T[:, kt, tcols],
                                   start=(kt == 0), stop=(kt == 7), skip_group_check=True, r=[f"wu{sl}"] + HK, w=[f"bank{pb}"])
                        op("pe", "matmul", out=bk(6)[:, 0:256], lhsT=sel16[0:16, e, :], rhs=gateT[0:16, tcols], start=True, stop=True,
                           r=["sel16"] + GK, w=["bank6"])
                        for ft in range(2):
                            pb = 4 + ft
                            op("act", "activation", out=sg[ft], in_=bk(pb)[:, 0:256], func=AF.Silu, r=[f"bank{pb}"], w=[f"sg{ft}"])
                            op("dve", "tensor_tensor", out=hu[ft], in0=sg[ft], in1=bk(pb)[:, 256:512], op=ALU.mult, r=[f"sg{ft}", f"bank{pb}"], w=[f"hu{ft}"])
                            op("dve", "tensor_tensor", out=hid[ft], in0=hu[ft], in1=bk(6)[:, 0:256], op=ALU.mult, r=[f"hu{ft}", "bank6"], w=[f"hid{ft}"])
                        for ft in range(2):
                            for t2 in range(2):
                                for dc in range(2):
                                    pob = t2 * 2 + dc
                                    op("pe", "matmul", out=bk(pob), lhsT=hid[ft][:, t2 * 128:(t2 + 1) * 128], rhs=wd_sb[sl][:, ft, dc * 512:(dc + 1) * 512],
                                       start=(ei == 0 and ft == 0), stop=(ei == 3 and ft == 1), r=[f"hid{ft}", f"wd{sl}"], w=[f"bank{pob}"])
                        if tg == n_tg - 1 and e + NSLOT < 16:
                            load_expert(e + NSLOT)
                    for t2 in range(2):
                        ti = tg * 2 + t2
                        for dc in range(2):
                            pob = t2 * 2 + dc
                            op("dve", "tensor_tensor", out=xtmp[:, dc * 512:(dc + 1) * 512], in0=bk(pob), in1=modbuf[:, 5, dc * 512:(dc + 1) * 512],
                               op=ALU.mult, r=[f"bank{pob}", "mod5"], w=[f"xtmp{dc}"])
                        op("pool", "tensor_tensor", out=xacc[t2], in0=xacc[t2], in1=xtmp, op=ALU.add, r=["xtmp0", "xtmp1", f"xacc{t2}"], w=[f"xacc{t2}"])
                        rows = slice(ti * 128, (ti + 1) * 128)
                        if G < 3:
                            dma("sp", xs_d[1][rows, :], xacc[t2], r=[f"xacc{t2}"], w=[f"xs1_{ti}"])
                        elif not last_layer:
                            dma("sp", xs_d[0][rows, :], xacc[t2], r=[f"xacc{t2}"], w=[f"xs0_{ti}"])
                            if "xout" in dbg:
                                dma("sp", dbg["xout"][rows, :], xacc[t2], r=[f"xacc{t2}"], w=["dbg_xout"])
                        else:
                            if "xout" in dbg and n_layers == 1:
                                dma("sp", dbg["xout"][rows, :], xacc[t2], r=[f"xacc{t2}"], w=["dbg_xout"])
                            op("act", "activation", out=xtmp, in_=xacc[t2], func=AF.Square, accum_out=fst[:, 0:1], r=[f"xacc{t2}"], w=["xtmp0", "xtmp1", "f_ss"])
                            op("dve", "tensor_scalar", out=fst[:, 1:2], in0=fst[:, 0:1], scalar1=1.0 / D, scalar2=EPS, op0=ALU.mult, op1=ALU.add, r=["f_ss"], w=["f_ms"])
                            op("pool", "tensor_tensor", out=fst[:, 3:4], in0=fst[:, 1:2], in1=negh, op=ALU.pow, r=["f_ms", "negh"], w=["f_rstd"])
                            op("dve", "scalar_tensor_tensor", out=xtmp, in0=xacc[t2], scalar=fst[:, 3:4], in1=gfin_bc, op0=ALU.mult, op1=ALU.mult,
                               r=[f"xacc{t2}", "f_rstd", "gfin_bc"], w=["xtmp0", "xtmp1"])
                            dma("sp", y_d[rows, :], xtmp, r=["xtmp0", "xtmp1"], w=[f"y_{ti}"])
            mark('s3_done')

        if cut is not None:
            P.ops = P.ops[:marks[cut]]
        print("arena peak bytes", ar.peak, "ops", len(P.ops))
        tracks = ["pe", "act", "dve", "pool", "sp"] + [("lane", i) for i in range(N_LANES)]
        sems = {t: es.enter_context(nc.semaphore(f"sem{i}")) for i, t in enumerate(tracks)}
        P.finalize(sems)
        block = es.enter_context(nc.Block())

        @block.sync
        def _(e):
            P.emit_engine("sp", e, final_wait=True)

        @block.tensor
        def _(e):
            P.emit_engine("pe", e)

        @block.scalar
        def _(e):
            P.emit_engine("act", e)

        @block.vector
        def _(e):
            P.emit_engine("dve", e)

        @block.gpsimd
        def _(e):
            P.emit_engine("pool", e)
    return nc


def make_in_maps(inputs, n_cores=8):
    f = lambda a: np.ascontiguousarray(np.asarray(a))
    consts = _constants()
    shared = {
        "ada_w": f(inputs["ada_w"]), "ada_b": f(inputs["ada_b"]),
        "norm_mix_g": f(inputs["norm_mix_g"]), "norm_ffn_g": f(inputs["norm_ffn_g"]),
        "final_norm_g": f(inputs["final_norm_g"]).reshape(1, D),
        "w_in": f(inputs["w_in"]), "sgu_norm_g": f(inputs["sgu_norm_g"]),
        "w_sguT": f(np.asarray(inputs["w_sgu"]).transpose(0, 3, 1, 2)),
        "b_sguT": f(np.asarray(inputs["b_sgu"]).transpose(0, 2, 1)),
        "w_pool2": f(np.asarray(inputs["w_pool"]).transpose(0, 2, 1, 3)),
        "pool_scale": f(inputs["pool_scale"]),
        "w_cmp1": f(inputs["w_cmp1"]), "w_cmp2": f(inputs["w_cmp2"]),
        "cmp_peT": f(np.asarray(inputs["cmp_pe"]).transpose(0, 1, 3, 2)),
        "w_out": f(inputs["w_out"]), "router_w": f(inputs["router_w"]),
        "router_b": f(inputs["router_b"]).reshape(1, 16),
        "w_gate": f(inputs["w_gate"]), "w_up": f(inputs["w_up"]), "w_down": f(inputs["w_down"]),
    }
    shared.update(consts)
    x = np.asarray(inputs["x"])
    c = np.asarray(inputs["c"])
    pos = np.asarray(inputs["positions"]).astype(np.int32)
    maps = []
    for k in range(n_cores):
        b = k % 4
        m = dict(shared)
        m["x"] = f(x[b])
        m["c2"] = f(c[b].reshape(8, 128).T)
        m["pos2"] = f(pos[b].reshape(32, 128).T)
        pc = np.zeros(256, np.int32)
        pc[:255] = pos[b][np.arange(255) * 16 + 16]
        m["posc2"] = f(pc.reshape(2, 128).T)
        maps.append(m)
    return maps


def kernel(**inputs):
    nc = build_program()
    maps = make_in_maps(inputs)
    res = run_bass_kernel_spmd(nc, maps, core_ids=list(range(8)))
    out = np.stack([res.results[b]["y"] for b in range(4)], axis=0)
    return out.astype(np.float32)
```
